# Optimizing a Trainium2 kernel written in Bass

```python
import jax, jax.numpy as jnp
from jax import lax
import numpy as np

D_MODEL = 1024
BATCH = 4
SEQ = 4096
DEPTH = 2

CHUNK = 64
D_MIX = D_MODEL
NH_M = 4
DH_M = 128
D_M = NH_M * DH_M
D_R = D_MIX - D_M
NB_R = 8
DB_R = D_R // NB_R
CONV_W = 4
LRU_C = 8.0
D_FF = 2816
N_EXPERTS = 8
TOP_K = 2
D_FF_E = 2816
MOE_BLOCK = 128
D_PLE = 256
EPS = 1e-6
N_DENSE = (DEPTH + 1) // 2
N_MOE = DEPTH // 2
Q_END = D_M
K_END = 2 * D_M
V_END = 3 * D_M
O_END = 4 * D_M
I_END = O_END + NH_M
F_END = I_END + NH_M
XR_END = F_END + D_R
D_IN = XR_END + D_R

kernel_name = "hymba_mlstm_rglru_moe_ple"


def group_rmsnorm(x, g, n_groups):
    xf = x.astype(jnp.float32)
    xg = xf.reshape(*x.shape[:-1], n_groups, x.shape[-1] // n_groups)
    xg = xg * lax.rsqrt(jnp.mean(xg * xg, axis=-1, keepdims=True) + EPS)
    return (xg.reshape(x.shape) * g.astype(jnp.float32)).astype(x.dtype)


def rmsnorm(x, g):
    return group_rmsnorm(x, g, 1)


def causal_conv(x, w, b):
    S = x.shape[1]
    xp = jnp.pad(x, ((0, 0), (CONV_W - 1, 0), (0, 0)))
    return sum(xp[:, j:j + S] * w[j] for j in range(CONV_W)) + b


def mlstm_chunkwise(q, k, v, i_pre, f_pre):
    B, S, H, Dh = q.shape
    L = CHUNK
    NC = S // L
    f32 = jnp.float32

    def blk(t):
        t = t.astype(f32).reshape(B, NC, L, *t.shape[2:])
        return jnp.moveaxis(t, 2, 3)

    qc = blk(q) * (Dh ** -0.5)
    kc, vc = blk(k), blk(v)
    ig = blk(i_pre)
    b = jnp.cumsum(blk(jax.nn.log_sigmoid(f_pre.astype(f32))), axis=-1)
    b_last = b[..., -1]
    a = b_last[..., None] - b + ig

    def step(carry, xs):
        C, n, m = carry
        bl, a_c, k_c, v_c = xs
        m_new = jnp.maximum(bl + m, a_c.max(-1))
        decay = jnp.exp(bl + m - m_new)
        wk = jnp.exp(a_c - m_new[..., None])
        C_new = decay[..., None, None] * C + jnp.einsum('bhl,bhld,bhle->bhde', wk, k_c, v_c)
        n_new = decay[..., None] * n + jnp.einsum('bhl,bhld->bhd', wk, k_c)
        return (C_new, n_new, m_new), (C, n, m)

    init = (jnp.zeros((B, H, Dh, Dh), f32), jnp.zeros((B, H, Dh), f32), jnp.zeros((B, H), f32))
    xs = (jnp.moveaxis(b_last, 1, 0), jnp.moveaxis(a, 1, 0),
          jnp.moveaxis(kc, 1, 0), jnp.moveaxis(vc, 1, 0))
    _, (C_prev, n_prev, m_prev) = lax.scan(step, init, xs)
    C_prev = jnp.moveaxis(C_prev, 0, 1)
    n_prev = jnp.moveaxis(n_prev, 0, 1)
    m_prev = jnp.moveaxis(m_prev, 0, 1)

    causal = jnp.tril(jnp.ones((L, L), dtype=bool))
    logD = jnp.where(causal, b[..., :, None] - b[..., None, :] + ig[..., None, :], -jnp.inf)
    m_inter = b + m_prev[..., None]
    m_row = jnp.maximum(m_inter, logD.max(-1))
    s = jnp.einsum('bchld,bchmd->bchlm', qc, kc) * jnp.exp(logD - m_row[..., None])
    inter = jnp.exp(m_inter - m_row)
    num = (jnp.einsum('bchlm,bchme->bchle', s, vc)
           + inter[..., None] * jnp.einsum('bchld,bchde->bchle', qc, C_prev))
    den = s.sum(-1) + inter * jnp.einsum('bchld,bchd->bchl', qc, n_prev)
    h = num / jnp.maximum(jnp.abs(den), jnp.exp(-m_row))[..., None]
    h = jnp.moveaxis(h, 3, 2).reshape(B, S, H, Dh)
    return h.astype(q.dtype)


def rglru(x, w_a, b_a, w_x, b_x, lam):
    B, S, _ = x.shape
    xf = x.astype(jnp.float32)
    xb = xf.reshape(B, S, NB_R, DB_R)
    r = jax.nn.sigmoid(jnp.einsum('bsnd,nde->bsne', xb, w_a.astype(jnp.float32)).reshape(B, S, D_R) + b_a)
    i = jax.nn.sigmoid(jnp.einsum('bsnd,nde->bsne', xb, w_x.astype(jnp.float32)).reshape(B, S, D_R) + b_x)
    log_a = -LRU_C * r * jax.nn.softplus(-lam.astype(jnp.float32))
    a = jnp.exp(log_a)
    u = jnp.sqrt(-jnp.expm1(2.0 * log_a)) * (i * xf)

    def combine(left, right):
        a_l, u_l = left
        a_r, u_r = right
        return a_l * a_r, a_r * u_l + u_r

    _, h = lax.associative_scan(combine, (a, u), axis=1)
    return h.astype(x.dtype)


def hybrid_mixer(hn, w_in, b_in, w_conv_qk, b_conv_qk, g_mh, w_conv_r, b_conv_r,
                 w_ra, b_ra, w_ri, b_ri, lam, g_r, w_out):
    B, S, _ = hn.shape
    z = hn @ w_in + b_in
    qk = jax.nn.silu(causal_conv(z[..., :K_END], w_conv_qk, b_conv_qk))
    q = qk[..., :D_M].reshape(B, S, NH_M, DH_M)
    k = qk[..., D_M:].reshape(B, S, NH_M, DH_M)
    v = z[..., K_END:V_END].reshape(B, S, NH_M, DH_M)
    o = jax.nn.sigmoid(z[..., V_END:O_END])
    hm = mlstm_chunkwise(q, k, v, z[..., O_END:I_END], z[..., I_END:F_END]).reshape(B, S, D_M) * o
    hm = group_rmsnorm(hm, g_mh, NH_M)
    xr = causal_conv(z[..., F_END:XR_END], w_conv_r, b_conv_r)
    hr = rglru(xr, w_ra, b_ra, w_ri, b_ri, lam) * jax.nn.gelu(z[..., XR_END:], approximate=True)
    hr = group_rmsnorm(hr, g_r, NB_R)
    return jnp.concatenate([hm, hr], axis=-1) @ w_out


def swiglu(x, w_gate, w_up, w_down):
    return (jax.nn.silu(x @ w_gate) * (x @ w_up)) @ w_down


def moe_swiglu(x, w_router, w_gate, w_up, w_down):
    B, S, D = x.shape
    N = B * S
    A = N * TOP_K
    xt = x.reshape(N, D)
    logits = xt.astype(jnp.float32) @ w_router.astype(jnp.float32)
    top_val, top_idx = lax.top_k(logits, TOP_K)
    gates = jax.nn.softmax(top_val, axis=-1)
    e_flat = top_idx.reshape(A)
    tok_flat = jnp.repeat(jnp.arange(N, dtype=jnp.int32), TOP_K)
    g_flat = gates.reshape(A)
    order = jnp.argsort(e_flat)
    e_sorted, tok_sorted, g_sorted = e_flat[order], tok_flat[order], g_flat[order]
    counts = jnp.bincount(e_flat, length=N_EXPERTS)
    padded = (counts + MOE_BLOCK - 1) // MOE_BLOCK * MOE_BLOCK
    p_end = jnp.cumsum(padded)
    p_start = p_end - padded
    u_start = jnp.cumsum(counts) - counts
    dest = p_start[e_sorted] + jnp.arange(A, dtype=jnp.int32) - u_start[e_sorted]
    P = A + N_EXPERTS * MOE_BLOCK
    n_blocks = P // MOE_BLOCK
    row_tok = jnp.full((P,), N, dtype=jnp.int32).at[dest].set(tok_sorted)
    row_gate = jnp.zeros((P,), jnp.float32).at[dest].set(g_sorted)
    block_expert = jnp.minimum(
        jnp.searchsorted(p_end, jnp.arange(n_blocks, dtype=jnp.int32) * MOE_BLOCK, side='right'),
        N_EXPERTS - 1)
    xpad = jnp.concatenate([xt, jnp.zeros((1, D), xt.dtype)], axis=0)

    def expert_block(args):
        toks, e = args
        xb = xpad[toks]
        return (jax.nn.silu(xb @ w_gate[e]) * (xb @ w_up[e])) @ w_down[e]

    y_rows = lax.map(expert_block, (row_tok.reshape(n_blocks, MOE_BLOCK), block_expert)).reshape(P, D)
    out = jnp.zeros((N + 1, D), jnp.float32).at[row_tok].add(y_rows.astype(jnp.float32) * row_gate[:, None])
    return out[:N].reshape(B, S, D).astype(x.dtype)


def setup_inputs(seed: int = 0) -> dict:
    key = jax.random.key(seed)
    ks = iter(jax.random.split(key, 40))
    nrm = lambda shape, scale: jax.random.normal(next(ks), shape, jnp.float32) * scale
    gain = lambda shape: 1.0 + nrm(shape, 0.02)
    a0 = jax.random.uniform(next(ks), (DEPTH, D_R), jnp.float32, 0.9, 0.999)
    s = a0 ** (1.0 / LRU_C)
    lam = jnp.log(s) - jnp.log1p(-s)
    f_bias = 3.0 + 3.0 * jax.random.uniform(next(ks), (DEPTH, NH_M), jnp.float32)
    b_in = nrm((DEPTH, D_IN), 0.02).at[:, I_END:F_END].set(f_bias)
    return {
        "x": nrm((BATCH, SEQ, D_MODEL), 1.0),
        "p": nrm((DEPTH, BATCH, SEQ, D_PLE), 1.0),
        "g_mix": gain((DEPTH, D_MODEL)),
        "w_in": nrm((DEPTH, D_MODEL, D_IN), D_MODEL ** -0.5),
        "b_in": b_in,
        "w_conv_qk": nrm((DEPTH, CONV_W, 2 * D_M), CONV_W ** -0.5),
        "b_conv_qk": nrm((DEPTH, 2 * D_M), 0.02),
        "g_mh": gain((DEPTH, D_M)),
        "w_conv_r": nrm((DEPTH, CONV_W, D_R), CONV_W ** -0.5),
        "b_conv_r": nrm((DEPTH, D_R), 0.02),
        "w_ra": nrm((DEPTH, NB_R, DB_R, DB_R), DB_R ** -0.5),
        "b_ra": nrm((DEPTH, D_R), 0.02),
        "w_ri": nrm((DEPTH, NB_R, DB_R, DB_R), DB_R ** -0.5),
        "b_ri": nrm((DEPTH, D_R), 0.02),
        "lam": lam,
        "g_r": gain((DEPTH, D_R)),
        "w_out": nrm((DEPTH, D_MIX, D_MODEL), D_MIX ** -0.5),
        "g_ffn": gain((DEPTH, D_MODEL)),
        "w_ff_gate": nrm((N_DENSE, D_MODEL, D_FF), D_MODEL ** -0.5),
        "w_ff_up": nrm((N_DENSE, D_MODEL, D_FF), D_MODEL ** -0.5),
        "w_ff_down": nrm((N_DENSE, D_FF, D_MODEL), D_FF ** -0.5),
        "w_router": nrm((N_MOE, D_MODEL, N_EXPERTS), D_MODEL ** -0.5),
        "w_e_gate": nrm((N_MOE, N_EXPERTS, D_MODEL, D_FF_E), D_MODEL ** -0.5),
        "w_e_up": nrm((N_MOE, N_EXPERTS, D_MODEL, D_FF_E), D_MODEL ** -0.5),
        "w_e_down": nrm((N_MOE, N_EXPERTS, D_FF_E, D_MODEL), D_FF_E ** -0.5),
        "g_ple": gain((DEPTH, D_MODEL)),
        "w_ple_gate": nrm((DEPTH, D_MODEL, D_MODEL), D_MODEL ** -0.5),
        "w_ple_proj": nrm((DEPTH, D_PLE, D_MODEL), D_PLE ** -0.5),
        "g_final": gain((D_MODEL,)),
    }


def reference(x, p, g_mix, w_in, b_in, w_conv_qk, b_conv_qk, g_mh, w_conv_r, b_conv_r,
              w_ra, b_ra, w_ri, b_ri, lam, g_r, w_out, g_ffn, w_ff_gate, w_ff_up, w_ff_down,
              w_router, w_e_gate, w_e_up, w_e_down, g_ple, w_ple_gate, w_ple_proj, g_final):
    h = x
    for i in range(DEPTH):
        hn = rmsnorm(h, g_mix[i])
        h = h + hybrid_mixer(hn, w_in[i], b_in[i], w_conv_qk[i], b_conv_qk[i], g_mh[i],
                             w_conv_r[i], b_conv_r[i], w_ra[i], b_ra[i], w_ri[i], b_ri[i],
                             lam[i], g_r[i], w_out[i])
        hn = rmsnorm(h, g_ffn[i])
        j = i // 2
        if i % 2 == 0:
            h = h + swiglu(hn, w_ff_gate[j], w_ff_up[j], w_ff_down[j])
        else:
            h = h + moe_swiglu(hn, w_router[j], w_e_gate[j], w_e_up[j], w_e_down[j])
        gate = jax.nn.sigmoid(rmsnorm(h, g_ple[i]) @ w_ple_gate[i])
        h = h + gate * (p[i] @ w_ple_proj[i])
    return rmsnorm(h, g_final)
```

```python
import contextlib
import math
import numpy as np
import concourse.bass as bass
import concourse.mybir as mybir
from concourse.bass_utils import run_bass_kernel_spmd

F32 = mybir.dt.float32
BF16 = mybir.dt.bfloat16
AF = mybir.ActivationFunctionType
ALU = mybir.AluOpType
AX = mybir.AxisListType

NCORES = 8
D = 1024
T = 2048
TB = 512
NTB = T // TB
KC = D // 128
DIN = 3080
DFF = 2816
NE = 8
DPLE = 256
EPS = 1e-6
SQRT_DH = math.sqrt(128.0)
SAME_ENGINE_SYNC = True
PAIRS = True
NDMA_SLOTS = 8

ENGS = ["pe", "act", "dve", "pool", "sp"]
DEBUG_LABELS = False
NO_WAR = False
NOWAR_KEEP = ("ps", "hT")
_HELPERS = {"op", "dma", "mm", "tr", "act", "tt", "ts", "stt", "copy", "recip", "load_w", "<lambda>"}


class Op:
    __slots__ = ("eng", "fn", "deps", "sig", "seq", "dma", "dma_idx", "pos", "waits", "gidx", "cost", "seg",
                 "t_done", "sched", "table", "label", "t_start", "crit")


class Sched:
    def __init__(self):
        self.ops = {e: [] for e in ENGS}
        self.lastw = {}
        self.readers = {}
        self.ndma = {e: 0 for e in ENGS}
        self.n = 0
        self.bar = []
        self.dmas = {e: [] for e in ENGS}
        self.ncc = 0
        self.seg = 0

    def barrier(self):
        self.seg += 1
        b = []
        for e in ENGS:
            comp = [o for o in self.ops[e] if not o.dma]
            if comp:
                b.append(comp[-1])
            b.extend(self.dmas[e][-NDMA_SLOTS:])
        self.bar = b

    def op(self, eng, fn, R=(), W=(), dma=False, cost=300):
        o = Op()
        o.cost = cost
        o.label = None
        if DEBUG_LABELS:
            import sys as _sys
            f = _sys._getframe(1)
            while f is not None and f.f_code.co_name in _HELPERS:
                f = f.f_back
            o.label = (f.f_code.co_name, f.f_lineno) if f is not None else None
        o.table = None
        o.seg = self.seg
        o.eng = eng
        o.fn = fn
        o.dma = dma
        o.sig = False
        o.seq = 0
        o.gidx = self.n
        self.n += 1
        deps = set(self.bar)
        for r in R:
            w = self.lastw.get(r)
            if w is not None:
                deps.add(w)
        for r in W:
            if NO_WAR and not (isinstance(r, tuple) and r[0] in NOWAR_KEEP):
                continue
            w = self.lastw.get(r)
            if w is not None:
                deps.add(w)
            for rd in self.readers.get(r, ()):
                deps.add(rd)
        for r in R:
            self.readers.setdefault(r, []).append(o)
        for r in W:
            self.lastw[r] = o
            self.readers[r] = []
        deps.discard(o)
        o.deps = deps
        if dma == "cc":
            o.dma_idx = self.ncc
            self.ncc += 1
        elif dma:
            o.dma_idx = self.ndma[eng]
            self.ndma[eng] += 1
            self.dmas[eng].append(o)
        o.pos = len(self.ops[eng])
        self.ops[eng].append(o)
        return o

    def reorder(self, window=48):
        new = {e: [] for e in ENGS}
        nseg = self.seg + 1
        byseg = {e: [[] for _ in range(nseg)] for e in ENGS}
        for e in ENGS:
            for o in self.ops[e]:
                o.sched = False
                o.t_done = 0.0
                byseg[e][o.seg].append(o)
        tnow = 0.0
        for sg in range(nseg):
            pend = {e: byseg[e][sg] for e in ENGS}
            head = {e: 0 for e in ENGS}
            free = {e: tnow for e in ENGS}
            cur_table = [None]
            remaining = sum(len(v) for v in pend.values())
            while remaining:
                best = None
                for e in ENGS:
                    lst = pend[e]
                    h = head[e]
                    while h < len(lst) and lst[h].sched:
                        h += 1
                    head[e] = h
                    if h >= len(lst):
                        continue
                    keep_order = (e == "sp")
                    cnt = 0
                    i = h
                    while i < len(lst) and cnt < window:
                        o = lst[i]
                        i += 1
                        if o.sched:
                            continue
                        cnt += 1
                        ok = True
                        tr = free[e]
                        cr = None
                        for d in o.deps:
                            if d.seg == sg and not d.sched:
                                ok = False
                                break
                            td = d.t_done + (60.0 if d.eng == e else 250.0)
                            if td > tr:
                                tr = td
                                cr = d
                        if ok and o.table is not None and o.table != cur_table[0]:
                            tr += 1300.0
                        if ok and (best is None or tr < best[0]):
                            best = (tr, e, o, cr)
                        if keep_order or o.dma:
                            break
                tr, e, o, cr = best
                o.t_start = tr
                o.crit = cr if cr is not None else (new[e][-1] if new[e] else None)
                if o.table is not None:
                    cur_table[0] = o.table
                o.sched = True
                o.t_done = tr + o.cost
                free[e] = tr + (o.cost if not o.dma else 100.0)
                new[e].append(o)
                remaining -= 1
            tnow = max([tnow] + [o.t_done for e in ENGS for o in pend[e]])
        self.ops = new
        self.est_ns = tnow
        for e in ENGS:
            k = 0
            for i, o in enumerate(self.ops[e]):
                o.pos = i
                if o.dma and o.dma != "cc":
                    o.dma_idx = k
                    k += 1

    def finalize(self):
        for e in ENGS:
            for o in self.ops[e]:
                latest = {}
                dmadeps = []
                for d in o.deps:
                    if d.dma:
                        dmadeps.append(d)
                        continue
                    if d.eng == o.eng and not o.dma and (o.eng == "pe" or not SAME_ENGINE_SYNC):
                        continue
                    cur = latest.get(d.eng)
                    if cur is None or d.pos > cur.pos:
                        latest[d.eng] = d
                for d in latest.values():
                    d.sig = True
                o.waits = (list(latest.values()), dmadeps)
        for e in ENGS:
            s = 0
            for o in self.ops[e]:
                if o.sig and not o.dma:
                    s += 1
                    o.seq = s

    def emit(self, nc, block, stack):
        sems = {e: stack.enter_context(nc.semaphore("s_" + e)) for e in ENGS}
        dsems = {}
        for e in ENGS:
            if self.ndma[e]:
                dsems[e] = [stack.enter_context(nc.semaphore("d_%s_%d" % (e, i)))
                            for i in range(NDMA_SLOTS)]
        ccsems = [stack.enter_context(nc.semaphore("cc_%d" % i)) for i in range(self.ncc)]
        binders = {"pe": block.tensor, "act": block.scalar, "dve": block.vector,
                   "pool": block.gpsimd, "sp": block.sync}

        def make(e):
            def body(eng):
                waited = {}

                def wait(key, sem, val):
                    if waited.get(key, 0) < val:
                        eng.wait_ge(sem, val)
                        waited[key] = val

                for o in self.ops[e]:
                    comp, dmas = o.waits
                    for d in comp:
                        wait(("c", d.eng), sems[d.eng], d.seq)
                    for d in dmas:
                        if d.dma == "cc":
                            wait(("cc", d.dma_idx), ccsems[d.dma_idx], 1)
                            continue
                        slot = d.dma_idx % NDMA_SLOTS
                        wait(("d", d.eng, slot), dsems[d.eng][slot], 16 * (d.dma_idx // NDMA_SLOTS + 1))
                    if o.dma == "cc":
                        ins = o.fn(eng)
                        ins.then_inc(ccsems[o.dma_idx], 1)
                        wait(("cc", o.dma_idx), ccsems[o.dma_idx], 1)
                    elif o.dma:
                        slot = o.dma_idx % NDMA_SLOTS
                        if o.dma_idx >= NDMA_SLOTS:
                            wait(("d", e, slot), dsems[e][slot], 16 * (o.dma_idx // NDMA_SLOTS))
                        ins = o.fn(eng)
                        ins.then_inc(dsems[e][slot], 16 if o.dma != "cc" else 16)
                    else:
                        ins = o.fn(eng)
                        if o.sig:
                            ins.then_inc(sems[e], 1)
                n = self.ndma[e]
                for slot in range(min(n, NDMA_SLOTS)):
                    cnt = (n - 1 - slot) // NDMA_SLOTS + 1
                    wait(("d", e, slot), dsems[e][slot], 16 * cnt)
            return body

        for e in ENGS:
            if self.ops[e]:
                binders[e](make(e))


def _cvec_layout():
    cols = {}
    n = 0

    def add(name, w):
        nonlocal n
        cols[name] = n
        n += w
    for l in range(2):
        add("g_mix%d" % l, 8)
        add("g_ffn%d" % l, 8)
        add("g_ple%d" % l, 8)
        add("b_q%d" % l, 4)
        add("b_k%d" % l, 4)
        add("b_xr%d" % l, 4)
        add("b_yr%d" % l, 4)
        add("cw_qk%d" % l, 32)
        add("cb_qk%d" % l, 8)
        add("cw_r%d" % l, 16)
        add("cb_r%d" % l, 4)
        add("b_ra%d" % l, 4)
        add("b_ri%d" % l, 4)
        add("lam%d" % l, 4)
        add("g_r%d" % l, 4)
    add("g_final", 8)
    add("eps", 1)
    add("one", 1)
    add("sel", 8)
    return cols, n


CV, NCV = _cvec_layout()
CR = {}
_n = 0
for _l in range(2):
    CR["b_tok%d" % _l] = _n
    _n += 1032
    CR["g_mh%d" % _l] = _n
    _n += 512
NCR = _n


def _col(v):
    return np.ascontiguousarray(np.asarray(v, np.float32).reshape(-1, 128).T)


def pack_consts(inp, core):
    cv = np.zeros((128, NCV), np.float32)

    def put(name, arr):
        arr = np.asarray(arr, np.float32)
        cv[:, CV[name]:CV[name] + arr.shape[1]] = arr
    for l in range(2):
        put("g_mix%d" % l, _col(inp["g_mix"][l]))
        put("g_ffn%d" % l, _col(inp["g_ffn"][l]))
        put("g_ple%d" % l, _col(inp["g_ple"][l]))
        b = inp["b_in"][l]
        put("b_q%d" % l, _col(b[0:512]))
        put("b_k%d" % l, _col(b[512:1024]))
        put("b_xr%d" % l, _col(b[2056:2568]))
        put("b_yr%d" % l, _col(b[2568:3080]))
        put("cw_qk%d" % l, np.concatenate([_col(inp["w_conv_qk"][l][j]) for j in range(4)], axis=1))
        put("cb_qk%d" % l, _col(inp["b_conv_qk"][l]))
        put("cw_r%d" % l, np.concatenate([_col(inp["w_conv_r"][l][j]) for j in range(4)], axis=1))
        put("cb_r%d" % l, _col(inp["b_conv_r"][l]))
        put("b_ra%d" % l, _col(inp["b_ra"][l]))
        put("b_ri%d" % l, _col(inp["b_ri"][l]))
        put("lam%d" % l, _col(inp["lam"][l]))
        put("g_r%d" % l, _col(inp["g_r"][l]))
    put("g_final", _col(inp["g_final"]))
    cv[:, CV["eps"]] = EPS
    cv[:, CV["one"]] = 1.0
    if core % 2 == 1:
        cv[:, CV["sel"] + (0 if PAIRS else core - 1)] = 1.0
    cr = np.zeros((128, NCR), np.float32)
    for l in range(2):
        cr[:, CR["b_tok%d" % l]:CR["b_tok%d" % l] + 1032] = np.asarray(inp["b_in"][l][1024:2056], np.float32)[None, :]
        cr[:, CR["g_mh%d" % l]:CR["g_mh%d" % l] + 512] = np.asarray(inp["g_mh"][l], np.float32)[None, :]
    return cv, cr


def const_mats():
    idx = np.arange(128)
    ident = np.eye(128, dtype=np.float32)
    tri = (idx[:, None] <= idx[None, :]).astype(np.float32)
    bo = ((idx[:, None] // 64) == (idx[None, :] // 64)).astype(np.float32)
    ones = np.ones((128, 128), np.float32)
    return np.concatenate([ident, tri, bo, ones], axis=1)


class Builder:
    def __init__(self, cfg):
        self.cfg = cfg
        self.nc = bass.Bass("TRN2", target_bir_lowering=False)
        self.S = Sched()
        self._mixer_w_off = 6400
        self._mixer_w_loaded = {}
        self.stack = contextlib.ExitStack()
        self.dram = {}
        self._uid = 0

    DSHAPES = {
        "x": [T, D], "p": [2, T, DPLE], "w_in": [2, D, DIN], "w_out": [2, D, D],
        "w_ff_gate": [D, DFF], "w_ff_up": [D, DFF], "w_ff_down": [DFF, D], "w_router": [D, NE],
        "w_e_gate": [NE, D, DFF], "w_e_up": [NE, D, DFF], "w_e_down": [NE, DFF, D],
        "w_ple_gate": [2, D, D], "w_ple_proj": [2, DPLE, D], "w_ra": [2, 8, 64, 64], "w_ri": [2, 8, 64, 64],
        "cvec": [128, NCV], "crow": [128, NCR], "cmat": [128, 512],
    }

    def g(self, name):
        if name not in self.dram:
            self.din(name, self.DSHAPES[name])
        return self.dram[name]

    def din(self, name, shape, dt=F32):
        t = self.nc.dram_tensor(name, list(shape), dt, kind="ExternalInput").ap()
        self.dram[name] = t
        return t

    def dout(self, name, shape, dt=F32):
        t = self.nc.dram_tensor(name, list(shape), dt, kind="ExternalOutput").ap()
        self.dram[name] = t
        return t

    def dscratch(self, name, shape, dt=F32):
        return self.nc.dram_tensor(name, list(shape), dt).ap()

    def sb(self, name, shape, dt=F32):
        return self.stack.enter_context(self.nc.sbuf_tensor(name, list(shape), dt))

    def psum(self, name, shape, dt=F32):
        return self.stack.enter_context(self.nc.psum_tensor(name, list(shape), dt))

    @staticmethod
    def fsz(ap):
        n = 1
        for d in ap.shape[1:]:
            n *= int(d)
        return n

    def op(self, eng, fn, R=(), W=()):
        return self.S.op(eng, fn, R, W)

    def dma(self, q, out, in_, R=(), W=()):
        nbytes = 128 * self.fsz(out) * 4
        return self.S.op(q, lambda e: e.dma_start(out=out, in_=in_), R, W, dma=True, cost=2500 + nbytes / 150.0)

    def mm(self, out, lhsT, rhs, start, stop, R=(), W=()):
        n = self.fsz(rhs)
        c = 64 + 0.45 * n
        if rhs.dtype == F32:
            c = 100 + 1.8 * n
        return self.S.op("pe", lambda e: e.matmul(out, lhsT, rhs, start=start, stop=stop), R, W, cost=c)

    def tr(self, out, in_, ident, R=(), W=()):
        return self.S.op("pe", lambda e: e.transpose(out, in_, ident), R, W, cost=150)

    def act(self, out, in_, func, R=(), W=(), bias=None, scale=None, accum_out=None):
        kw = {}
        if bias is not None:
            kw["bias"] = bias
        if scale is not None:
            kw["scale"] = scale
        if accum_out is not None:
            kw["accum_out"] = accum_out
        o = self.S.op("act", lambda e: e.activation(out, in_, func, **kw), R, W, cost=260 + 0.75 * self.fsz(out))
        if func in (AF.Exp, AF.Ln, AF.Sigmoid, AF.Silu, AF.Sqrt):
            o.table = func
        return o

    def tt(self, eng, out, in0, in1, op, R=(), W=()):
        c = (80 + 1.05 * self.fsz(out)) if eng == "dve" else (200 + 2.1 * self.fsz(out))
        return self.S.op(eng, lambda e: e.tensor_tensor(out, in0, in1, op), R, W, cost=c)

    def ts(self, eng, out, in0, s1, s2, op0, op1=None, R=(), W=()):
        c = (80 + 1.05 * self.fsz(out)) if eng == "dve" else (200 + 2.1 * self.fsz(out))
        if op1 is None:
            return self.S.op(eng, lambda e: e.tensor_scalar(out, in0, s1, None, op0), R, W, cost=c)
        return self.S.op(eng, lambda e: e.tensor_scalar(out, in0, s1, s2, op0, op1), R, W, cost=c)

    def stt(self, out, in0, scalar, in1, op0, op1, R=(), W=()):
        return self.S.op("dve", lambda e: e.scalar_tensor_tensor(out, in0, scalar, in1, op0, op1), R, W,
                         cost=80 + 1.6 * self.fsz(out))

    def copy(self, eng, out, in_, R=(), W=()):
        if eng == "act":
            return self.S.op("act", lambda e: e.copy(out, in_), R, W, cost=260 + 0.75 * self.fsz(out))
        c = (80 + 1.05 * self.fsz(out)) if eng == "dve" else (200 + 2.1 * self.fsz(out))
        return self.S.op(eng, lambda e: e.tensor_copy(out, in_), R, W, cost=c)

    def recip(self, out, in_, R=(), W=()):
        return self.S.op("dve", lambda e: e.reciprocal(out, in_), R, W, cost=100 + 4.0 * self.fsz(out))

    def setup(self):
        cfg = self.cfg
        if cfg.get("mode") != "A":
            self.out = self.dout("out", [T, D])

        sb = self.sb
        self.hT = sb("hT", [128, KC * T], F32)
        self.hT3 = self.hT[:].rearrange("p (c t) -> p c t", c=KC)
        self.cvec = sb("cvec_s", [128, NCV], F32)
        self.cmat = sb("cmat_s", [128, 512], F32)
        self.ident_f = self.cmat[:, 0:128]
        self.tri_f = self.cmat[:, 128:256]
        self.bo_f = self.cmat[:, 256:384]
        self.ones_f = self.cmat[:, 384:512]
        self.ident_b = sb("ident_b", [128, 128], BF16)
        self.ones_b = sb("ones_b", [128, 128], BF16)
        self.ps = [self.psum("ps%d" % i, [128, 512], F32) for i in range(8)]
        self.sq = [sb("sq%d" % i, [128, TB], BF16) for i in range(2)]
        self.rs = sb("rs", [128, TB], F32)
        BIGW = cfg.get("big_words", 34 * 1024)
        self.BIGW = BIGW
        self.big = sb("big", [128, BIGW], F32)
        self.xin = [self.big[:, BIGW - 4096 + i * 1024:BIGW - 4096 + (i + 1) * 1024] for i in range(2)]
        self.hn3 = self.big[:, BIGW - 2048:BIGW].bitcast(BF16).rearrange("p (c t) -> p c t", c=KC)

        self.dma("sp", self.cvec[:], self.g("cvec")[:, :], W=["cvec"])
        self.dma("sp", self.cmat[:], self.g("cmat")[:, :], W=["cmat"])
        self.copy("dve", self.ident_b[:], self.ident_f, R=["cmat"], W=["ident_b"])
        self.copy("dve", self.ones_b[:], self.ones_f, R=["cmat"], W=["ones_b"])

    def cv(self, name, j=0, w=1):
        c = CV[name] + j
        return self.cvec[:, c:c + w]

    def hres(self, t0, t1):
        return [("hT", i) for i in range(t0 // 128, (t1 + 127) // 128)]

    def load_x(self):
        for tt in range(T // 128):
            xb = self.xin[tt % 2]
            r = ("xin", tt % 2)
            self.dma("sp", xb, self.g("x")[tt * 128:(tt + 1) * 128, :], W=[r])
            for half in range(2):
                bank = 6 + half
                for j in range(4):
                    c = half * 4 + j
                    self.tr(self.ps[bank][:, j * 128:(j + 1) * 128], xb[:, c * 128:(c + 1) * 128],
                            self.ident_f, R=[r, "cmat"], W=[("ps", bank)])
                eng = "dve" if half == 0 else "act"
                self.copy(eng, self.hT3[:, half * 4:half * 4 + 4, tt * 128:(tt + 1) * 128],
                          self.ps[bank][:].rearrange("p (c t) -> p c t", c=4),
                          R=[("ps", bank)], W=[("hT", tt)])

    def store_out(self):
        for tt in range(T // 128):
            xb = self.xin[tt % 2]
            r = ("xin", tt % 2)
            for half in range(2):
                bank = 6 + half
                for j in range(4):
                    c = half * 4 + j
                    self.tr(self.ps[bank][:, j * 128:(j + 1) * 128], self.hT3[:, c, tt * 128:(tt + 1) * 128],
                            self.ident_f, R=[("hT", tt), "cmat"], W=[("ps", bank)])
                eng = "dve" if half == 0 else "act"
                self.copy(eng, xb[:, half * 512:(half + 1) * 512], self.ps[bank][:],
                          R=[("ps", bank)], W=[r])
            self.dma("sp", self.out[tt * 128:(tt + 1) * 128, :], xb, R=[r], W=[("out", tt)])

    def norm_stats(self, t0, n, bank=6):
        hr = self.hres(t0, t0 + n)
        for c in range(KC):
            sq = self.sq[c % 2]
            self.act(sq[:, :n], self.hT3[:, c, t0:t0 + n], AF.Square, R=hr, W=[("sq", c % 2)])
            self.mm(self.ps[bank][:, :n], self.ones_b[:], sq[:, :n], c == 0, c == KC - 1,
                    R=[("sq", c % 2), "ones_b"], W=[("ps", bank)])
        self.act(self.rs[:, :n], self.ps[bank][:, :n], AF.Sqrt, R=[("ps", bank), "cvec"], W=["rs"],
                 bias=self.cv("eps"), scale=1.0 / D)
        self.recip(self.rs[:, :n], self.rs[:, :n], R=["rs"], W=["rs"])

    def norm_apply(self, t0, n, gname, dst3, dres):
        hr = self.hres(t0, t0 + n)
        for c in range(KC):
            self.stt(dst3[:, c, :n], self.hT3[:, c, t0:t0 + n], self.cv(gname, c), self.rs[:, :n],
                     ALU.mult, ALU.mult, R=hr + ["rs", "cvec"], W=[dres])

    def final_norm(self):
        dbg = self.cfg.get("dbg")
        if dbg is not None:
            hr = self.hres(0, TB)
            if dbg == "act":
                self.act(self.sq[0][:, :TB], self.hT3[:, 0, 0:TB], AF.Square, R=hr, W=[("sq", 0)])
            elif dbg == "dve":
                self.ts("dve", self.hT3[:, 0, 0:TB], self.hT3[:, 0, 0:TB], 1.0, None, ALU.mult, R=hr, W=hr)
            elif dbg == "dve2":
                self.ts("dve", self.rs[:, 0:TB], self.hT3[:, 0, 0:TB], 1.0, None, ALU.mult, R=hr, W=["rs"])
            elif dbg == "dve3":
                self.ts("dve", self.rs[:, 0:TB], self.rs[:, 0:TB], 1.0, None, ALU.mult, R=[], W=["rs"])
            elif dbg == "dve4":
                self.copy("dve", self.rs[:, 0:TB], self.rs[:, 0:TB], R=[], W=["rs"])
            elif dbg == "dve5":
                sv = self.S.bar
                self.S.bar = []
                self.ts("dve", self.rs[:, 0:TB], self.rs[:, 0:TB], 1.0, None, ALU.mult, R=[], W=["rs"])
                self.S.bar = sv
            elif dbg == "dve6":
                self.stt(self.rs[:, 0:TB], self.rs[:, 0:TB], 1.0, self.rs[:, 0:TB], ALU.mult, ALU.mult, R=[], W=["rs"])
            elif dbg.startswith("dveN"):
                for i in range(int(dbg[4:])):
                    self.copy("dve", self.rs[:, 0:TB], self.rs[:, 0:TB], R=[], W=["rs"])
            elif dbg.startswith("actN"):
                for i in range(int(dbg[4:])):
                    self.act(self.sq[0][:, :TB], self.hT3[:, 0, 0:TB], AF.Square, R=hr, W=[("sq", 0)])
            elif dbg == "pe":
                self.mm(self.ps[6][:, :TB], self.ones_b[:], self.ones_b[:], True, True, R=["ones_b"], W=[("ps", 6)])
            elif dbg == "pool":
                self.ts("pool", self.hT3[:, 0, 0:TB], self.hT3[:, 0, 0:TB], 1.0, None, ALU.mult, R=hr, W=hr)
            return
        for tb in range(NTB):
            t0 = tb * TB
            self.norm_stats(t0, TB)
            hr = self.hres(t0, t0 + TB)
            for c in range(KC):
                self.stt(self.hT3[:, c, t0:t0 + TB], self.hT3[:, c, t0:t0 + TB], self.cv("g_final", c),
                         self.rs[:, :TB], ALU.mult, ALU.mult, R=hr + ["rs", "cvec"], W=hr)

    def load_w(self, dst, src, res):
        return self.dma("pool", dst, src, W=[res])

    def ple(self, l):
        big = self.big
        o = self._mixer_w_off + KC * DIN // 2 + KC * D // 2
        if l == 0 and self.cfg.get("prefetch_next", True) and ("mixer", 1) in self.cfg["phases"]:
            self.mixer_prefetch(1)
        wpg = big[:, o:o + 4096].bitcast(BF16).rearrange("p (k n) -> p k n", k=KC)
        o += 4096
        wpp = big[:, o:o + 1024].bitcast(BF16).rearrange("p (k n) -> p k n", k=2)
        o += 1024
        pst = big[:, o:o + 1024].rearrange("p (i n) -> p i n", i=4)
        o += 1024
        pT = big[:, o:o + 512].bitcast(BF16).rearrange("p (j t) -> p j t", j=2)
        o += 512
        assert o <= self.BIGW - 4096
        sg = [big[:, i * 512:(i + 1) * 512] for i in range(2)]
        tmp = [big[:, 1024 + i * 512:1024 + (i + 1) * 512] for i in range(2)]
        self.load_w(wpg, self.g("w_ple_gate")[l].rearrange("(k p) n -> p k n", p=128), "wpg")
        self.load_w(wpp, self.g("w_ple_proj")[l].rearrange("(k p) n -> p k n", p=128), "wpp")
        for tb in range(NTB):
            t0 = tb * TB
            hr = self.hres(t0, t0 + TB)
            self.dma("sp", pst, self.g("p")[l, t0:t0 + TB, :].rearrange("(i q) n -> q i n", q=128), W=["pst"])
            self.norm_stats(t0, TB)
            self.norm_apply(t0, TB, "g_ple%d" % l, self.hn3, "hn")
            for j in range(2):
                for i in range(4):
                    self.tr(self.ps[7][:, i * 128:(i + 1) * 128], pst[:, i, j * 128:(j + 1) * 128],
                            self.ident_f, R=["pst", "cmat"], W=[("ps", 7)])
                self.copy("act", pT[:, j, :], self.ps[7][:], R=[("ps", 7)], W=["pT"])
            for fo in range(KC):
                a = fo % 2
                b = 2 + fo % 2
                for kc in range(KC):
                    self.mm(self.ps[a][:], wpg[:, kc, fo * 128:(fo + 1) * 128], self.hn3[:, kc, :],
                            kc == 0, kc == KC - 1, R=["wpg", "hn"], W=[("ps", a)])
                for j in range(2):
                    self.mm(self.ps[b][:], wpp[:, j, fo * 128:(fo + 1) * 128], pT[:, j, :],
                            j == 0, j == 1, R=["wpp", "pT"], W=[("ps", b)])
                self.act(sg[fo % 2], self.ps[a][:], AF.Sigmoid, R=[("ps", a)], W=[("sg", fo % 2)])
                self.tt("dve", tmp[fo % 2], self.ps[b][:], sg[fo % 2], ALU.mult,
                        R=[("ps", b), ("sg", fo % 2)], W=[("ptmp", fo % 2)])
                self.tt("pool", self.hT3[:, fo, t0:t0 + TB], self.hT3[:, fo, t0:t0 + TB], tmp[fo % 2], ALU.add,
                        R=hr + [("ptmp", fo % 2)], W=hr)

    def ffn_norm_all(self, gname, hn_all3):
        for tb in range(NTB):
            t0 = tb * TB
            self.norm_stats(t0, TB)
            hr = self.hres(t0, t0 + TB)
            for c in range(KC):
                self.stt(hn_all3[:, c, t0:t0 + TB], self.hT3[:, c, t0:t0 + TB], self.cv(gname, c),
                         self.rs[:, :TB], ALU.mult, ALU.mult, R=hr + ["rs", "cvec"], W=[("hna", tb)])
            if self.cfg.get("moe_hook") is not None and gname.startswith("g_ffn1"):
                self.cfg["moe_hook"](tb)

    def ffn_core(self, experts, hn_all3, o, gate_fn=None):
        big = self.big
        GS = [(0, 4), (4, 4), (8, 4), (12, 4), (16, 4), (20, 2)]
        wgb, wub, wdb = [], [], []
        for i in range(2):
            wgb.append(big[:, o:o + 2048].bitcast(BF16).rearrange("p (k n) -> p k n", k=KC))
            o += 2048
            wub.append(big[:, o:o + 2048].bitcast(BF16).rearrange("p (k n) -> p k n", k=KC))
            o += 2048
            wdb.append(big[:, o:o + 2048].bitcast(BF16).rearrange("p (j n) -> p j n", j=4))
            o += 2048
        actb = []
        for i in range(2):
            actb.append(big[:, o:o + 1024].bitcast(BF16).rearrange("p (j t) -> p j t", j=4))
            o += 1024
        st = [big[:, o + i * 512:o + (i + 1) * 512] for i in range(2)]
        o += 1024
        st2 = [big[:, o + i * 256:o + (i + 1) * 256].bitcast(BF16) for i in range(2)]
        o += 512
        self._ffn_end = o

        steps = []
        gi = 0
        for e, (wg, wu, wd) in enumerate(experts):
            for (c0, nch) in GS:
                for tb in range(NTB):
                    steps.append((e, gi, c0, nch, tb))
                gi += 1

        def load_group(e, g, c0, nch):
            wg, wu, wd = experts[e]
            par = g % 2
            n = nch * 128
            self.load_w(wgb[par][:, :, :n], wg[:, c0 * 128:c0 * 128 + n].rearrange("(k p) n -> p k n", p=128),
                        ("wgb", par))
            self.load_w(wub[par][:, :, :n], wu[:, c0 * 128:c0 * 128 + n].rearrange("(k p) n -> p k n", p=128),
                        ("wub", par))
            self.load_w(wdb[par][:, :nch, :], wd[c0 * 128:c0 * 128 + n, :].rearrange("(j p) f -> p j f", p=128),
                        ("wdb", par))

        groups = []
        for e in range(len(experts)):
            for (c0, nch) in GS:
                groups.append((e, len(groups), c0, nch))
        load_group(*groups[0])

        def down(step, sidx):
            e, g, c0, nch, tb = step
            par = g % 2
            t0 = tb * TB
            hr = self.hres(t0, t0 + TB)
            ab = actb[sidx % 2]
            for fo in range(KC):
                bank = 4 + fo % 2
                for j in range(nch):
                    self.mm(self.ps[bank][:], wdb[par][:, j, fo * 128:(fo + 1) * 128], ab[:, j, :],
                            j == 0, j == nch - 1, R=[("wdb", par), ("actb", sidx % 2)], W=[("ps", bank)])
                self.tt("dve", self.hT3[:, fo, t0:t0 + TB], self.hT3[:, fo, t0:t0 + TB], self.ps[bank][:], ALU.add,
                        R=hr + [("ps", bank)], W=hr)

        prev = None
        for sidx, step in enumerate(steps):
            e, g, c0, nch, tb = step
            par = g % 2
            t0 = tb * TB
            ab = actb[sidx % 2]
            gate = gate_fn(e) if gate_fn is not None else None
            if gate_fn is not None and c0 == 4 and tb == 0:
                gate_fn(e + 1)
            for j in range(nch):
                a = j % 2
                b = 2 + j % 2
                for kc in range(KC):
                    self.mm(self.ps[a][:], wgb[par][:, kc, j * 128:(j + 1) * 128], hn_all3[:, kc, t0:t0 + TB],
                            kc == 0, kc == KC - 1, R=[("wgb", par), ("hna", tb)], W=[("ps", a)])
                for kc in range(KC):
                    self.mm(self.ps[b][:], wub[par][:, kc, j * 128:(j + 1) * 128], hn_all3[:, kc, t0:t0 + TB],
                            kc == 0, kc == KC - 1, R=[("wub", par), ("hna", tb)], W=[("ps", b)])
                if gate is None:
                    self.act(st2[j % 2], self.ps[a][:], AF.Silu, R=[("ps", a)], W=[("st2", j % 2)])
                else:
                    gap, gres = gate
                    self.act(st[j % 2], self.ps[a][:], AF.Silu, R=[("ps", a)], W=[("st", j % 2)])
                    self.tt("pool", st2[j % 2], st[j % 2], gap[:, t0:t0 + TB], ALU.mult,
                            R=[("st", j % 2), gres], W=[("st2", j % 2)])
                self.tt("dve", ab[:, j, :], self.ps[b][:], st2[j % 2], ALU.mult,
                        R=[("ps", b), ("st2", j % 2)], W=[("actb", sidx % 2)])
            if prev is not None:
                down(*prev)
            prev = (step, sidx)
            if tb == 0 and g + 1 < len(groups):
                load_group(*groups[g + 1])
        down(*prev)

    def ffn_dense(self):
        big = self.big
        hn_all3 = big[:, 0:8192].bitcast(BF16).rearrange("p (c t) -> p c t", c=KC)
        self.ffn_norm_all("g_ffn0", hn_all3)
        self.ffn_core([(self.g("w_ff_gate"), self.g("w_ff_up"), self.g("w_ff_down"))], hn_all3, 8192)


    def moe(self):
        big = self.big
        hn_all3 = big[:, 0:8192].bitcast(BF16).rearrange("p (c t) -> p c t", c=KC)
        o = 8192
        wr = big[:, o:o + 64].rearrange("p (k e) -> p k e", k=KC)
        o += 64
        gates = big[:, o:o + 128].rearrange("p (t e) -> p t e", t=16)
        o += 128
        sm = big[:, o:o + 64]
        o += 64
        diag = [big[:, o + i * 512:o + (i + 1) * 512] for i in range(2)]
        o += 1024
        grep = [big[:, o + i * 1024:o + (i + 1) * 1024].bitcast(BF16) for i in range(2)]
        o += 2048
        lgt, eq1, lg2, eq2 = sm[:, 0:9], sm[:, 16:24], sm[:, 24:32], sm[:, 32:40]
        m1, m2, dd, g1, g2 = sm[:, 40:41], sm[:, 41:42], sm[:, 42:43], sm[:, 43:44], sm[:, 44:45]

        self.dma("sp", wr, self.g("w_router").rearrange("(k p) e -> p k e", p=128), W=["wr"])
        for k in range(KC):
            self.ts("dve", wr[:, k, :], wr[:, k, :], self.cv("g_ffn1", k), None, ALU.mult,
                    R=["wr", "cvec"], W=["wr"])

        def route(tb):
            for i in range(4):
                tt = tb * 4 + i
                hr = [("hT", tt)]
                for kc in range(KC):
                    self.mm(self.ps[7][:, 0:8], self.hT3[:, kc, tt * 128:(tt + 1) * 128], wr[:, kc, :],
                            kc == 0, kc == KC - 1, R=hr + ["wr"], W=[("ps", 7)])
                self.mm(self.ps[7][:, 8:9], self.rs[0:1, i * 128:(i + 1) * 128], self.ones_f[0:1, 0:1],
                        True, True, R=["rs", "cmat"], W=[("ps", 7)])
                self.copy("dve", lgt, self.ps[7][:, 0:9], R=[("ps", 7)], W=["sm"])
                S = self.S
                S.op("dve", lambda e: e.reduce_max(m1, lgt[:, 0:8], AX.X), ["sm"], ["sm"])
                self.ts("dve", eq1, lgt[:, 0:8], m1, None, ALU.is_equal, R=["sm"], W=["sm"])
                self.stt(lg2, eq1, -1e30, lgt[:, 0:8], ALU.mult, ALU.add, R=["sm"], W=["sm"])
                S.op("dve", lambda e: e.reduce_max(m2, lg2, AX.X), ["sm"], ["sm"])
                self.ts("dve", eq2, lg2, m2, None, ALU.is_equal, R=["sm"], W=["sm"])
                self.ts("dve", dd, m1, m2, lgt[:, 8:9], ALU.subtract, ALU.mult, R=["sm"], W=["sm"])
                self.act(g1, dd, AF.Sigmoid, R=["sm"], W=["sm"])
                self.ts("dve", g2, g1, -1.0, 1.0, ALU.mult, ALU.add, R=["sm"], W=["sm"])
                self.ts("dve", gates[:, tt, :], eq1, g1, None, ALU.mult, R=["sm"], W=["gates"])
                self.stt(gates[:, tt, :], eq2, g2, gates[:, tt, :], ALU.mult, ALU.add, R=["sm", "gates"], W=["gates"])

        self.cfg["moe_hook"] = route
        self.ffn_norm_all("g_ffn1", hn_all3)
        self.cfg["moe_hook"] = None

        built = {}

        def gate_fn(e):
            if e in built or e >= NE:
                return built.get(e)
            gp = grep[e % 2]
            res = ("grep", e % 2)
            for q in range(4):
                dg = diag[q % 2]
                for j in range(4):
                    tt = q * 4 + j
                    self.ts("pool", dg[:, j * 128:(j + 1) * 128], self.ident_f, gates[:, tt, e:e + 1], None, ALU.mult,
                            R=["gates", "cmat"], W=[("diag", q % 2)])
                self.mm(self.ps[7][:], self.ones_f, dg, True, True, R=[("diag", q % 2), "cmat"], W=[("ps", 7)])
                self.copy("act", gp[:, q * 512:(q + 1) * 512], self.ps[7][:], R=[("ps", 7)], W=[res])
            built[e] = (gp, res)
            return built[e]

        wg, wu, wd = self.g("w_e_gate"), self.g("w_e_up"), self.g("w_e_down")
        nexp = self.cfg.get("n_experts", NE)
        self.ffn_core([(wg[e], wu[e], wd[e]) for e in range(nexp)], hn_all3, o, gate_fn)


    def exchange(self, l, tag, src_ap, W_, dst_ap):
        NR = 2 if self.cfg.get("pairs", True) else NCORES
        groups = [[2 * i, 2 * i + 1] for i in range(NCORES // 2)] if self.cfg.get("pairs", True) else [list(range(NCORES))]
        ccs = self.dscratch("ccs_%s%d" % (tag, l), [128, W_])
        ccd = self.dscratch("ccd_%s%d" % (tag, l), [NR * 128, W_])
        gath = self.big[:, 0:NR * W_].rearrange("p (r w) -> p r w", r=NR)
        self.S.barrier()
        if self.cfg.get("no_cc"):
            self.ts("dve", dst_ap, src_ap, 0.0, None, ALU.mult, R=["xsrc"], W=["xdst"])
            self.S.barrier()
            return
        self.dma("pool", ccs[:, :], src_ap, R=["xsrc"], W=["ccs"])
        if not self.cfg.get("skip_cc"):
            self.S.op("pool", lambda e: e.collective_compute("AllGather", ALU.bypass,
                                                             replica_groups=groups,
                                                             ins=[ccs[:, :]], outs=[ccd[:, :]]),
                      R=["ccs"], W=["ccd"], dma="cc")
        qg = self.cfg.get("gq", "pool")
        self.dma(qg, gath, ccd.rearrange("(r p) w -> p r w", r=NR), R=["ccd"], W=["gath"])
        self.ts("dve", dst_ap, gath[:, 0, :], self.cv("sel", 0), None, ALU.mult, R=["gath", "cvec"], W=["xdst"])
        for r in range(1, NR):
            self.stt(dst_ap, gath[:, r, :], self.cv("sel", r), dst_ap, ALU.mult, ALU.add,
                     R=["gath", "cvec", "xdst"], W=["xdst"])
        self.S.barrier()

    def mixer_prefetch(self, l):
        big = self.big
        o0 = self._mixer_w_off
        win = big[:, o0:o0 + KC * DIN // 2].bitcast(BF16).rearrange("p (k n) -> p k n", k=KC)
        o1 = o0 + KC * DIN // 2
        wout = big[:, o1:o1 + KC * D // 2].bitcast(BF16).rearrange("p (k n) -> p k n", k=KC)
        self.load_w(win, self.g("w_in")[l].rearrange("(k p) n -> p k n", p=128), "win")
        self.load_w(wout, self.g("w_out")[l].rearrange("(k p) n -> p k n", p=128), "wout")
        self._mixer_w_loaded[l] = True

    def mixer(self, l):
        TM = 256
        big = self.big
        off = [0]

        def take(words):
            a = big[:, off[0]:off[0] + words]
            off[0] += words
            return a
        rg_sets = [[take(TM) for _ in range(8)] for _ in range(2)]
        P_t = take(512)
        erow = take(256).bitcast(BF16)
        Sw = take(256).bitcast(BF16)
        qs = take(256).bitcast(BF16)
        kw = take(256).bitcast(BF16)
        hmo = take(512)
        trilf = hmo
        tmpo = hmo
        hmn = take(256).bitcast(BF16)
        assert off[0] >= NCORES * 520
        xpack = big[:, 2 * 556:3 * 556]
        xrecv = big[:, 3 * 556:4 * 556]
        assert off[0] == self._mixer_w_off, (off[0], self._mixer_w_off)
        win = take(KC * DIN // 2).bitcast(BF16).rearrange("p (k n) -> p k n", k=KC)
        wout = take(KC * D // 2).bitcast(BF16).rearrange("p (k n) -> p k n", k=KC)
        bda = take(512).rearrange("p (c n) -> p c n", c=4)
        bdi = take(512).rearrange("p (c n) -> p c n", c=4)
        crow = take(1544)
        b_tok = crow[:, 0:1032]
        g_mh = crow[:, 1032:1544]
        halo0 = take(36).rearrange("p (j n) -> p j n", j=12)
        halo = take(36).rearrange("p (j n) -> p j n", j=12)
        tailb = take(36).rearrange("p (j n) -> p j n", j=12)
        state = take(520)
        Caug = state[:, 0:516].rearrange("p (h n) -> p h n", h=4)
        rstate = state[:, 516:520]
        state_in = take(520)
        Cbf = take(258).bitcast(BF16).rearrange("p (h n) -> p h n", h=4)
        kap = take(4)
        kap2 = take(4)
        smx = take(48)
        qT = take(4 * TM // 2).bitcast(BF16).rearrange("p (h t) -> p h t", h=4)
        kT = take(4 * TM // 2).bitcast(BF16).rearrange("p (h t) -> p h t", h=4)
        hmT = take(4 * TM // 2).bitcast(BF16).rearrange("p (h t) -> p h t", h=4)
        hrn = take(4 * TM // 2).bitcast(BF16).rearrange("p (h t) -> p h t", h=4)
        zraw = [take(TM + 4) for _ in range(2)]
        accb = [take(TM) for _ in range(2)]
        vaug = [take(258).bitcast(BF16).rearrange("p (h n) -> p h n", h=4) for _ in range(2)]
        osig = take(256).bitcast(BF16)
        ifg = take(8)
        assert off[0] <= self.BIGW - 4096, (off[0], self.BIGW)
        hn3 = self.hn3
        ps = self.ps
        bctr = [0]

        def nb():
            b = bctr[0] % 8
            bctr[0] += 1
            return b
        L = str(l)
        w_in = self.g("w_in")[l]

        def cvl(name, j=0, w=1):
            return self.cv(name + L, j, w)

        if not self._mixer_w_loaded.get(l):
            self.mixer_prefetch(l)
        self.dma("sp", crow, self.g("crow")[:, l * 1544:(l + 1) * 1544], W=["crow"])
        self.op("pool", lambda e: e.memset(bda, 0.0), W=["bda"])
        self.op("pool", lambda e: e.memset(bdi, 0.0), W=["bdi"])
        for par in range(2):
            lo = par * 64
            self.dma("sp", bda[lo:lo + 64, :, lo:lo + 64],
                     self.g("w_ra")[l].rearrange("(c two) d e -> two d c e", two=2)[par], R=[], W=["bda"])
            self.dma("sp", bdi[lo:lo + 64, :, lo:lo + 64],
                     self.g("w_ri")[l].rearrange("(c two) d e -> two d c e", two=2)[par], R=[], W=["bdi"])
        for i in range(2):
            self.op("pool", lambda e, i=i: e.memset(vaug[i][:, :, 128:129], 1.0), W=[("vaug", i)])
        self.act(kap, cvl("lam", 0, 4), AF.Exp, R=["cvec"], W=["kap"], scale=-1.0)
        self.act(kap, kap, AF.Ln, R=["kap", "cvec"], W=["kap"], bias=self.cv("one"))
        self.ts("dve", kap2, kap, -16.0, None, ALU.mult, R=["kap"], W=["kap2"])
        self.ts("dve", kap, kap, -8.0, None, ALU.mult, R=["kap"], W=["kap"])

        def fm_desc(kind, c):
            if kind == "q":
                return (c, c * 128, cvl("b_q", c), [cvl("cw_qk", t * 8 + c) for t in range(4)], cvl("cb_qk", c))
            if kind == "k":
                return (4 + c, 512 + c * 128, cvl("b_k", c), [cvl("cw_qk", t * 8 + 4 + c) for t in range(4)],
                        cvl("cb_qk", 4 + c))
            return (8 + c, 2056 + c * 128, cvl("b_xr", c), [cvl("cw_r", t * 4 + c) for t in range(4)], cvl("cb_r", c))

        mode = self.cfg.get("mode", "F")
        if mode != "B":
            self.norm_stats(T - 128, 128, bank=nb())
            self.norm_apply(T - 128, 128, "g_mix" + L, hn3, "hn")
            n = 0
            for kind in ("q", "k", "x"):
                for c in range(4):
                    jdx, co, bias, taps, cb = fm_desc(kind, c)
                    bank = nb()
                    n += 1
                    for kc in range(KC):
                        self.mm(ps[bank][:, :128], win[:, kc, co:co + 128], hn3[:, kc, :128], kc == 0, kc == KC - 1,
                                R=["win", "hn"], W=[("ps", bank)])
                    self.act(tailb[:, jdx, :], ps[bank][:, 125:128], AF.Identity, R=[("ps", bank), "cvec"], W=["xsrc"],
                             bias=bias)
        if mode == "F":
            self.op("pool", lambda e: e.memset(halo0.rearrange("p j n -> p (j n)"), 0.0), W=["xdst"])
        elif mode == "A":
            o_tail = self.dout("o_tail", [128, 36])
            self.dma("sp", o_tail[:, :], tailb.rearrange("p j n -> p (j n)"), R=["xsrc"], W=["o_tail"])
            self.op("pool", lambda e: e.memset(halo0.rearrange("p j n -> p (j n)"), 0.0), W=["xdst"])
        else:
            i_tail = self.din("i_tail", [128, 36])
            self.dma("sp", halo0.rearrange("p j n -> p (j n)"), i_tail[:, :], W=["xdst"])

        cnt = {"fm": 0}

        def conv_chunk(kind, c, t0):
            jdx, co, bias, taps, cb = fm_desc(kind, c)
            rot = cnt["fm"] % 2
            cnt["fm"] += 1
            bank = nb()
            for kc in range(KC):
                self.mm(ps[bank][:, :TM], win[:, kc, co:co + 128], hn3[:, kc, :TM], kc == 0, kc == KC - 1,
                        R=["win", "hn"], W=[("ps", bank)])
            zr = zraw[rot]
            zres = ("zraw", rot)
            hres_ = ("halo", jdx)
            self.copy("pool", zr[:, 0:3], halo[:, jdx, :], R=[hres_], W=[zres])
            self.act(zr[:, 3:3 + TM], ps[bank][:, :TM], AF.Identity, R=[("ps", bank), "cvec"], W=[zres], bias=bias)
            self.copy("pool", halo[:, jdx, :], zr[:, TM:TM + 3], R=[zres], W=[hres_])
            acc = accb[rot]
            ares = ("acc", rot)
            self.ts("dve", acc, zr[:, 3:3 + TM], taps[3], cb, ALU.mult, ALU.add, R=[zres, "cvec"], W=[ares])
            for t in (2, 1, 0):
                self.stt(acc, zr[:, t:t + TM], taps[t], acc, ALU.mult, ALU.add, R=[zres, ares, "cvec"], W=[ares])
            return acc, ares

        def rglru_chunk(c, xr, xres, state_only, t0):
            r_t, ig_t, a_t, a2_t, u_t, hr_t, gel_t, x2_t = rg_sets[c % 2]
            sfx = "%d" % (c % 2)
            bA, bI = nb(), nb()
            self.mm(ps[bA][:, :TM], bda[:, c, :], xr, True, True, R=["bda", xres], W=[("ps", bA)])
            self.mm(ps[bI][:, :TM], bdi[:, c, :], xr, True, True, R=["bdi", xres], W=[("ps", bI)])
            self.act(r_t, ps[bA][:, :TM], AF.Sigmoid, R=[("ps", bA), "cvec"], W=["r_t" + sfx], bias=cvl("b_ra", c))
            self.act(ig_t, ps[bI][:, :TM], AF.Sigmoid, R=[("ps", bI), "cvec"], W=["ig_t" + sfx], bias=cvl("b_ri", c))
            self.act(a_t, r_t, AF.Exp, R=["r_t" + sfx, "kap"], W=["a_t" + sfx], scale=kap[:, c:c + 1])
            self.act(a2_t, r_t, AF.Exp, R=["r_t" + sfx, "kap2"], W=["a2_t" + sfx], scale=kap2[:, c:c + 1])
            self.act(a2_t, a2_t, AF.Sqrt, R=["a2_t" + sfx, "cvec"], W=["a2_t" + sfx], scale=-1.0, bias=self.cv("one"))
            self.tt("pool", ig_t, ig_t, xr, ALU.mult, R=["ig_t" + sfx, xres], W=["ig_t" + sfx])
            self.tt("pool", u_t, ig_t, a2_t, ALU.mult, R=["ig_t" + sfx, "a2_t" + sfx], W=["u_t" + sfx])
            self.S.op("dve", lambda e: e.tensor_tensor_scan(hr_t, a_t, u_t, rstate[:, c:c + 1], ALU.mult, ALU.add),
                      R=["a_t" + sfx, "u_t" + sfx, "rstate"], W=["hr_t" + sfx])
            self.copy("act", rstate[:, c:c + 1], hr_t[:, TM - 1:TM], R=["hr_t" + sfx], W=["rstate"])
            if state_only:
                return
            co = 2568 + c * 128
            bank = nb()
            for kc in range(KC):
                self.mm(ps[bank][:, :TM], win[:, kc, co:co + 128], hn3[:, kc, :TM], kc == 0, kc == KC - 1,
                        R=["win", "hn"], W=[("ps", bank)])
            self.act(gel_t, ps[bank][:, :TM], AF.Identity, R=[("ps", bank), "cvec"], W=["gel_t" + sfx], bias=cvl("b_yr", c))
            self.act(x2_t, gel_t, AF.Square, R=["gel_t" + sfx], W=["x2_t" + sfx])
            self.ts("dve", x2_t, x2_t, 0.044715, 1.0, ALU.mult, ALU.add, R=["x2_t" + sfx], W=["x2_t" + sfx])
            self.tt("dve", x2_t, x2_t, gel_t, ALU.mult, R=["x2_t" + sfx, "gel_t" + sfx], W=["x2_t" + sfx])
            self.act(x2_t, x2_t, AF.Sigmoid, R=["x2_t" + sfx], W=["x2_t" + sfx], scale=1.5957691216057308)
            self.tt("pool", gel_t, gel_t, x2_t, ALU.mult, R=["gel_t" + sfx, "x2_t" + sfx], W=["gel_t" + sfx])
            self.tt("pool", gel_t, gel_t, hr_t, ALU.mult, R=["gel_t" + sfx, "hr_t" + sfx], W=["gel_t" + sfx])
            self.act(x2_t, gel_t, AF.Square, R=["gel_t" + sfx], W=["x2_t" + sfx])
            bG = nb()
            self.mm(ps[bG][:, :TM], self.bo_f, x2_t, True, True, R=["cmat", "x2_t" + sfx], W=[("ps", bG)])
            self.act(x2_t, ps[bG][:, :TM], AF.Sqrt, R=[("ps", bG), "cvec"], W=["x2_t" + sfx], scale=1.0 / 64, bias=self.cv("eps"))
            self.recip(x2_t, x2_t, R=["x2_t" + sfx], W=["x2_t" + sfx])
            self.stt(hrn[:, c, :], gel_t, cvl("g_r", c), x2_t, ALU.mult, ALU.mult, R=["gel_t" + sfx, "x2_t" + sfx, "cvec"], W=["hrn"])

        def tok_major(i, state_only, vi):
            va = vaug[vi]
            vres = ("vaug", vi)
            lhs = lambda kc: hn3[:, kc, i * 128:(i + 1) * 128]
            bv, bif = nb(), nb()
            for kc in range(KC):
                self.mm(ps[bv][:], lhs(kc), win[:, kc, 1024:1536], kc == 0, kc == KC - 1, R=["win", "hn"], W=[("ps", bv)])
            self.tt("dve", va[:, :, 0:128], ps[bv][:].rearrange("p (h n) -> p h n", h=4),
                    b_tok[:, 0:512].rearrange("p (h n) -> p h n", h=4), ALU.add, R=[("ps", bv), "crow"], W=[vres])
            for kc in range(KC):
                self.mm(ps[bif][:, 16:24], lhs(kc), win[:, kc, 2048:2056], kc == 0, kc == KC - 1, R=["win", "hn"], W=[("ps", bif)])
            self.tt("dve", ifg, ps[bif][:, 16:24], b_tok[:, 1024:1032], ALU.add, R=[("ps", bif), "crow"], W=["ifg"])
            if not state_only:
                bo = nb()
                for kc in range(KC):
                    self.mm(ps[bo][:], lhs(kc), win[:, kc, 1536:2048], kc == 0, kc == KC - 1, R=["win", "hn"], W=[("ps", bo)])
                self.tt("dve", tmpo, ps[bo][:], b_tok[:, 512:1024], ALU.add, R=[("ps", bo), "crow"], W=["hmo"])
                self.act(osig, tmpo, AF.Sigmoid, R=["hmo"], W=["osig"])

        e1, nlf, gm, gw, wk, dec = (smx[:, 0:4], smx[:, 4:8], smx[:, 8:12], smx[:, 12:16], smx[:, 16:20], smx[:, 20:24])
        den, rden, ss, rstd = smx[:, 24:28], smx[:, 28:32], smx[:, 32:36], smx[:, 36:40]
        tri4 = self.tri_f

        def mlstm_chunk(i, state_only, vi):
            va = vaug[vi]
            vres = ("vaug", vi)
            tc_ = slice(i * 128, (i + 1) * 128)
            b7 = nb()
            self.act(e1, ifg[:, 4:8], AF.Exp, R=["ifg"], W=["e1"], scale=-1.0)
            self.act(nlf, e1, AF.Ln, R=["e1", "cvec"], W=["nlf"], bias=self.cv("one"))
            self.mm(ps[b7][:, 0:4], self.tri_f, nlf, True, True, R=["cmat", "nlf"], W=[("ps", b7)])
            self.mm(ps[b7][:, 4:8], self.ones_f, nlf, True, True, R=["cmat", "nlf"], W=[("ps", b7)])
            self.tt("dve", gm, ifg[:, 0:4], ps[b7][:, 0:4], ALU.add, R=["ifg", ("ps", b7)], W=["gm"])
            self.tt("dve", gw, gm, ps[b7][:, 4:8], ALU.subtract, R=["gm", ("ps", b7)], W=["gw"])
            self.act(wk, gw, AF.Exp, R=["gw"], W=["wk"])
            self.act(dec, ps[b7][:, 4:8], AF.Exp, R=[("ps", b7)], W=["dec"], scale=-1.0)
            if not state_only:
                for h in range(4):
                    self.ts("dve", trilf[:, h * 128:(h + 1) * 128], self.tri_f, nlf[:, h:h + 1], None, ALU.mult,
                            R=["cmat", "nlf"], W=["hmo"])
                b6, b5 = nb(), nb()
                self.mm(ps[b6][:], self.ones_f, trilf, True, True, R=["cmat", "hmo"], W=[("ps", b6)])
                for h in range(4):
                    self.act(P_t[:, h * 128:(h + 1) * 128], ps[b6][:, h * 128:(h + 1) * 128], AF.Exp,
                             R=[("ps", b6), "gm"], W=["P_t"], scale=-1.0, bias=gm[:, h:h + 1])
                self.act(erow, ps[b6][:], AF.Exp, R=[("ps", b6)], W=["erow"], scale=-1.0)
                for h in range(4):
                    self.mm(ps[b5][:, h * 128:(h + 1) * 128], kT[:, h, tc_], qT[:, h, tc_], True, True,
                            R=["kT", "qT"], W=[("ps", b5)])
                for h in range(4):
                    self.tt("dve", P_t[:, h * 128:(h + 1) * 128], P_t[:, h * 128:(h + 1) * 128], self.tri_f, ALU.mult,
                            R=["P_t", "cmat"], W=["P_t"])
                self.tt("dve", Sw, ps[b5][:], P_t, ALU.mult, R=[("ps", b5), "P_t"], W=["Sw"])
                self.tt("pool", qs.rearrange("p (h n) -> p h n", h=4), qT[:, :, tc_],
                        erow.rearrange("p (h n) -> p h n", h=4), ALU.mult, R=["qT", "erow"], W=["qs"])
                bo2 = [nb(), nb()]
                for h in range(4):
                    b = bo2[h // 2]
                    o_ = (h % 2) * 129
                    self.mm(ps[b][:, o_:o_ + 129], Sw[:, h * 128:(h + 1) * 128], va[:, h, :], True, False,
                            R=["Sw", vres], W=[("ps", b)])
                    self.mm(ps[b][:, o_:o_ + 129], qs[:, h * 128:(h + 1) * 128], Cbf[:, h, :], False, True,
                            R=["qs", "Cbf"], W=[("ps", b)])
                for b in range(2):
                    v3 = ps[bo2[b]][:, 0:258].rearrange("p (h n) -> p h n", h=2)
                    self.act(den[:, 2 * b:2 * b + 2], v3[:, :, 128], AF.Abs, R=[("ps", bo2[b])], W=["den"])
                self.ts("dve", den, den, SQRT_DH, None, ALU.max, R=["den"], W=["den"])
                self.recip(rden, den, R=["den"], W=["rden"])
                for h in range(4):
                    b = bo2[h // 2]
                    o_ = (h % 2) * 129
                    self.stt(hmo[:, h * 128:(h + 1) * 128], ps[b][:, o_:o_ + 128], rden[:, h:h + 1],
                             osig[:, h * 128:(h + 1) * 128], ALU.mult, ALU.mult,
                             R=[("ps", b), "rden", "osig"], W=["hmo"])
                self.act(P_t, hmo, AF.Square, R=["hmo"], W=["P_t"])
                self.S.op("dve", lambda e: e.tensor_reduce(ss, P_t.rearrange("p (h n) -> p h n", h=4), AX.X, ALU.add),
                          R=["P_t"], W=["ss"])
                self.act(rstd, ss, AF.Sqrt, R=["ss", "cvec"], W=["rstd"], scale=1.0 / 128, bias=self.cv("eps"))
                self.recip(rstd, rstd, R=["rstd"], W=["rstd"])
                for h in range(4):
                    self.stt(hmn[:, h * 128:(h + 1) * 128], hmo[:, h * 128:(h + 1) * 128], rstd[:, h:h + 1],
                             g_mh[:, h * 128:(h + 1) * 128], ALU.mult, ALU.mult, R=["hmo", "rstd", "crow"], W=["hmn"])
                bT = nb()
                pTb = ps[bT][:].bitcast(BF16)
                for h in range(4):
                    self.tr(pTb[:, h * 128:(h + 1) * 128], hmn[:, h * 128:(h + 1) * 128], self.ident_b[:],
                            R=["hmn", "ident_b"], W=[("ps", bT)])
                self.copy("act", hmT[:, :, tc_], pTb[:, 0:512].rearrange("p (h n) -> p h n", h=4),
                          R=[("ps", bT)], W=["hmT"])
            bK = nb()
            pKb = ps[bK][:].bitcast(BF16)
            for h in range(4):
                self.tr(pKb[:, h * 128:(h + 1) * 128], kT[:, h, tc_], self.ident_b[:], R=["kT", "ident_b"], W=[("ps", bK)])
            for h in range(4):
                self.ts("dve", kw[:, h * 128:(h + 1) * 128], pKb[:, h * 128:(h + 1) * 128], wk[:, h:h + 1], None, ALU.mult,
                        R=[("ps", bK), "wk"], W=["kw"])
            bu2 = [nb(), nb()]
            for h in range(4):
                b = bu2[h // 2]
                o_ = (h % 2) * 129
                self.mm(ps[b][:, o_:o_ + 129], kw[:, h * 128:(h + 1) * 128], va[:, h, :], True, True,
                        R=["kw", vres], W=[("ps", b)])
            for h in range(4):
                b = bu2[h // 2]
                o_ = (h % 2) * 129
                self.stt(Caug[:, h, :], Caug[:, h, :], dec[:, h:h + 1], ps[b][:, o_:o_ + 129], ALU.mult, ALU.add,
                         R=["state", "dec", ("ps", b)], W=["state"])
            self.copy("act", Cbf, Caug, R=["state"], W=["Cbf"])

        def run_pass(state_only):
            self.copy("pool", halo.rearrange("p j n -> p (j n)"), halo0.rearrange("p j n -> p (j n)"),
                      R=["xdst"], W=[("halo", j) for j in range(12)])
            vcnt = 0
            for tb in range(T // TM):
                t0 = tb * TM
                hr = self.hres(t0, t0 + TM)
                self.norm_stats(t0, TM, bank=nb())
                self.norm_apply(t0, TM, "g_mix" + L, hn3, "hn")
                for c in range(4):
                    acc, ares = conv_chunk("k", c, t0)
                    self.act(kT[:, c, :], acc, AF.Silu, R=[ares], W=["kT"])
                if not state_only:
                    for c in range(4):
                        acc, ares = conv_chunk("q", c, t0)
                        self.act(qT[:, c, :], acc, AF.Silu, R=[ares], W=["qT"])
                for i in range(TM // 128):
                    vi = vcnt % 2
                    vcnt += 1
                    tok_major(i, state_only, vi)
                    mlstm_chunk(i, state_only, vi)
                for c in range(4):
                    acc, ares = conv_chunk("x", c, t0)
                    rglru_chunk(c, acc, ares, state_only, t0)
                if not state_only:
                    for fo in range(KC):
                        bank = nb()
                        for kc in range(KC):
                            rhs = hmT[:, kc, :] if kc < 4 else hrn[:, kc - 4, :]
                            self.mm(ps[bank][:, :TM], wout[:, kc, fo * 128:(fo + 1) * 128], rhs, kc == 0, kc == KC - 1,
                                    R=["wout", "hmT", "hrn"], W=[("ps", bank)])
                        self.tt("dve", self.hT3[:, fo, t0:t0 + TM], self.hT3[:, fo, t0:t0 + TM], ps[bank][:, :TM], ALU.add,
                                R=hr + [("ps", bank)], W=hr)

        self.op("pool", lambda e: e.memset(state, 0.0), W=["state", "rstate"])
        self.op("pool", lambda e: e.memset(Cbf, 0.0), W=["Cbf"])
        if mode == "A":
            run_pass(True)
            o_state = self.dout("o_state", [128, 520])
            self.dma("sp", o_state[:, :], state, R=["state", "rstate"], W=["o_state"])
            return
        if mode == "B":
            i_state = self.din("i_state", [128, 520])
            self.dma("sp", state, i_state[:, :], W=["state", "rstate", "xdst"])
            self.copy("pool", Cbf, Caug, R=["xdst", "state"], W=["Cbf"])
        elif self.cfg.get("prepass", True):
            run_pass(True)
            self.S.barrier()
            self.copy("pool", xpack[:, 0:520], state, R=["state", "rstate"], W=["xsrc"])
            self.copy("pool", xpack[:, 520:556], tailb.rearrange("p j n -> p (j n)"), R=["xsrc"], W=["xsrc"])
            self.exchange(l, "s", xpack, 556, xrecv)
            self.copy("pool", state, xrecv[:, 0:520], R=["xdst"], W=["state", "rstate"])
            self.copy("pool", halo0.rearrange("p j n -> p (j n)"), xrecv[:, 520:556], R=["xdst"], W=["xdst"])
            self.copy("pool", Cbf, Caug, R=["state"], W=["Cbf"])
            self.S.barrier()
        run_pass(False)

    def program(self):
        cfg = self.cfg
        self.setup()
        if cfg["phases"] and cfg["phases"][0] == ("mixer", 0):
            self.mixer_prefetch(0)
        self.load_x()
        for pi, ph in enumerate(cfg["phases"]):
            if not (pi == 0 and ph[0] == "mixer"):
                self.S.barrier()
            if ph[0] == "ple":
                self.ple(ph[1])
            elif ph[0] == "ffn":
                self.ffn_dense()
            elif ph[0] == "moe":
                self.moe()
            elif ph[0] == "mixer":
                self.mixer(ph[1])
            elif ph[0] == "xchg":
                src = self.big[:, 20000:20036]
                dst = self.big[:, 20100:20136]
                self.copy("pool", src, self.cvec[:, 0:36], R=["cvec"], W=["xsrc"])
                self.exchange(ph[1], "x", src, 36, dst)
        if not (cfg["phases"] and cfg["phases"][-1][0] == "ple"):
            self.S.barrier()
        if cfg.get("mode") != "A":
            if cfg.get("final_norm", True):
                self.final_norm()
            self.store_out()
        if cfg.get("reorder", True):
            self.S.reorder(cfg.get("window", 48))
        self.S.finalize()
        with self.nc.Block() as block:
            self.S.emit(self.nc, block, self.stack)
        self.stack.close()
        return self.nc


FULL_CFG = {"phases": [("mixer", 0), ("ffn",), ("ple", 0), ("mixer", 1), ("moe",), ("ple", 1)]}


def make_in_maps(inputs, x_override=None, extra=None):
    f = lambda a: np.ascontiguousarray(np.asarray(a, np.float32))
    x = f(inputs["x"] if x_override is None else x_override).reshape(4 * 4096, D)
    p = f(inputs["p"]).reshape(2, 4 * 4096, DPLE)
    shared = {
        "w_in": f(inputs["w_in"]), "w_out": f(inputs["w_out"]),
        "w_ff_gate": f(inputs["w_ff_gate"][0]), "w_ff_up": f(inputs["w_ff_up"][0]),
        "w_ff_down": f(inputs["w_ff_down"][0]), "w_router": f(inputs["w_router"][0]),
        "w_e_gate": f(inputs["w_e_gate"][0]), "w_e_up": f(inputs["w_e_up"][0]),
        "w_e_down": f(inputs["w_e_down"][0]), "w_ple_gate": f(inputs["w_ple_gate"]),
        "w_ple_proj": f(inputs["w_ple_proj"]), "w_ra": f(inputs["w_ra"]), "w_ri": f(inputs["w_ri"]),
        "cmat": const_mats(),
    }
    maps = []
    for c in range(NCORES):
        cv, cr = pack_consts(inputs, c)
        m = dict(shared)
        m["x"] = np.ascontiguousarray(x[c * T:(c + 1) * T])
        m["p"] = np.ascontiguousarray(p[:, c * T:(c + 1) * T])
        m["cvec"] = cv
        m["crow"] = cr
        if extra is not None:
            m.update(extra[c])
        maps.append(m)
    return maps


_NC_CACHE = {}


def run(inputs, cfg, trace=False, x_override=None, extra=None):
    key = repr(sorted((k, repr(v)) for k, v in cfg.items()))
    if key not in _NC_CACHE:
        b = Builder(dict(cfg))
        _NC_CACHE[key] = b.program()
        _NC_CACHE[key + "#names"] = set(b.dram.keys())
    nc = _NC_CACHE[key]
    maps = make_in_maps(inputs, x_override, extra)
    names = _NC_CACHE[key + "#names"]
    maps = [{k: v for k, v in m.items() if k in names} for m in maps]
    res = run_bass_kernel_spmd(nc, maps, core_ids=list(range(NCORES)), **({"trace": True} if trace else {}))
    if cfg.get("mode") == "A":
        return None, res
    out = np.concatenate([np.asarray(r["out"], np.float32) for r in res.results], axis=0)
    return out.reshape(4, 4096, D), res


def _carry(resA):
    extra = []
    for c in range(NCORES):
        if c % 2 == 1:
            extra.append({"i_tail": np.asarray(resA.results[c - 1]["o_tail"], np.float32),
                          "i_state": np.asarray(resA.results[c - 1]["o_state"], np.float32)})
        else:
            extra.append({"i_tail": np.zeros((128, 36), np.float32), "i_state": np.zeros((128, 520), np.float32)})
    return extra


def kernel_unfused(inputs):
    _, rA0 = run(inputs, {"phases": [("mixer", 0)], "mode": "A"})
    h1, _ = run(inputs, {"phases": [("mixer", 0), ("ffn",), ("ple", 0)], "mode": "B", "final_norm": False},
                extra=_carry(rA0))
    _, rA1 = run(inputs, {"phases": [("mixer", 1)], "mode": "A"}, x_override=h1)
    out, _ = run(inputs, {"phases": [("mixer", 1), ("moe",), ("ple", 1)], "mode": "B", "final_norm": True},
                 x_override=h1, extra=_carry(rA1))
    return out


def kernel(**inputs):
    out, _ = run(inputs, FULL_CFG)
    return out
```

```python
import contextlib
import math
import numpy as np
import concourse.bass as bass
import concourse.mybir as mybir
from concourse.bass_utils import run_bass_kernel_spmd

F32 = mybir.dt.float32
BF16 = mybir.dt.bfloat16
AF = mybir.ActivationFunctionType
ALU = mybir.AluOpType
AX = mybir.AxisListType

NCORES = 8
D = 1024
T = 2048
TB = 512
NTB = T // TB
KC = D // 128
DIN = 3080
DFF = 2816
NE = 8
DPLE = 256
EPS = 1e-6
SQRT_DH = math.sqrt(128.0)
SAME_ENGINE_SYNC = True
PAIRS = True
NDMA_SLOTS = 8

ENGS = ["pe", "act", "dve", "pool", "sp"]
DEBUG_LABELS = False
NO_WAR = False
NOWAR_KEEP = ("ps", "hT")
_HELPERS = {"op", "dma", "mm", "tr", "act", "tt", "ts", "stt", "copy", "recip", "load_w", "<lambda>"}


class Op:
    __slots__ = ("eng", "fn", "deps", "sig", "seq", "dma", "dma_idx", "pos", "waits", "gidx", "cost", "seg",
                 "t_done", "sched", "table", "label", "t_start", "crit")


class Sched:
    def __init__(self):
        self.ops = {e: [] for e in ENGS}
        self.lastw = {}
        self.readers = {}
        self.ndma = {e: 0 for e in ENGS}
        self.n = 0
        self.bar = []
        self.dmas = {e: [] for e in ENGS}
        self.ncc = 0
        self.seg = 0

    def barrier(self):
        self.seg += 1
        b = []
        for e in ENGS:
            comp = [o for o in self.ops[e] if not o.dma]
            if comp:
                b.append(comp[-1])
            b.extend(self.dmas[e][-NDMA_SLOTS:])
        self.bar = b

    def op(self, eng, fn, R=(), W=(), dma=False, cost=300):
        o = Op()
        o.cost = cost
        o.label = None
        if DEBUG_LABELS:
            import sys as _sys
            f = _sys._getframe(1)
            while f is not None and f.f_code.co_name in _HELPERS:
                f = f.f_back
            o.label = (f.f_code.co_name, f.f_lineno) if f is not None else None
        o.table = None
        o.seg = self.seg
        o.eng = eng
        o.fn = fn
        o.dma = dma
        o.sig = False
        o.seq = 0
        o.gidx = self.n
        self.n += 1
        deps = set(self.bar)
        for r in R:
            w = self.lastw.get(r)
            if w is not None:
                deps.add(w)
        for r in W:
            if NO_WAR and not (isinstance(r, tuple) and r[0] in NOWAR_KEEP):
                continue
            w = self.lastw.get(r)
            if w is not None:
                deps.add(w)
            for rd in self.readers.get(r, ()):
                deps.add(rd)
        for r in R:
            self.readers.setdefault(r, []).append(o)
        for r in W:
            self.lastw[r] = o
            self.readers[r] = []
        deps.discard(o)
        o.deps = deps
        if dma == "cc":
            o.dma_idx = self.ncc
            self.ncc += 1
        elif dma:
            o.dma_idx = self.ndma[eng]
            self.ndma[eng] += 1
            self.dmas[eng].append(o)
        o.pos = len(self.ops[eng])
        self.ops[eng].append(o)
        return o

    def reorder(self, window=48):
        new = {e: [] for e in ENGS}
        nseg = self.seg + 1
        byseg = {e: [[] for _ in range(nseg)] for e in ENGS}
        for e in ENGS:
            for o in self.ops[e]:
                o.sched = False
                o.t_done = 0.0
                byseg[e][o.seg].append(o)
        tnow = 0.0
        for sg in range(nseg):
            pend = {e: byseg[e][sg] for e in ENGS}
            head = {e: 0 for e in ENGS}
            free = {e: tnow for e in ENGS}
            cur_table = [None]
            remaining = sum(len(v) for v in pend.values())
            while remaining:
                best = None
                for e in ENGS:
                    lst = pend[e]
                    h = head[e]
                    while h < len(lst) and lst[h].sched:
                        h += 1
                    head[e] = h
                    if h >= len(lst):
                        continue
                    keep_order = (e == "sp")
                    cnt = 0
                    i = h
                    while i < len(lst) and cnt < window:
                        o = lst[i]
                        i += 1
                        if o.sched:
                            continue
                        cnt += 1
                        ok = True
                        tr = free[e]
                        cr = None
                        for d in o.deps:
                            if d.seg == sg and not d.sched:
                                ok = False
                                break
                            td = d.t_done + (60.0 if d.eng == e else 250.0)
                            if td > tr:
                                tr = td
                                cr = d
                        if ok and o.table is not None and o.table != cur_table[0]:
                            tr += 1300.0
                        if ok and (best is None or tr < best[0]):
                            best = (tr, e, o, cr)
                        if keep_order or o.dma:
                            break
                tr, e, o, cr = best
                o.t_start = tr
                o.crit = cr if cr is not None else (new[e][-1] if new[e] else None)
                if o.table is not None:
                    cur_table[0] = o.table
                o.sched = True
                o.t_done = tr + o.cost
                free[e] = tr + (o.cost if not o.dma else 100.0)
                new[e].append(o)
                remaining -= 1
            tnow = max([tnow] + [o.t_done for e in ENGS for o in pend[e]])
        self.ops = new
        self.est_ns = tnow
        for e in ENGS:
            k = 0
            for i, o in enumerate(self.ops[e]):
                o.pos = i
                if o.dma and o.dma != "cc":
                    o.dma_idx = k
                    k += 1

    def finalize(self):
        for e in ENGS:
            for o in self.ops[e]:
                latest = {}
                dmadeps = []
                for d in o.deps:
                    if d.dma:
                        dmadeps.append(d)
                        continue
                    if d.eng == o.eng and not o.dma and (o.eng == "pe" or not SAME_ENGINE_SYNC):
                        continue
                    cur = latest.get(d.eng)
                    if cur is None or d.pos > cur.pos:
                        latest[d.eng] = d
                for d in latest.values():
                    d.sig = True
                o.waits = (list(latest.values()), dmadeps)
        for e in ENGS:
            s = 0
            for o in self.ops[e]:
                if o.sig and not o.dma:
                    s += 1
                    o.seq = s

    def emit(self, nc, block, stack):
        sems = {e: stack.enter_context(nc.semaphore("s_" + e)) for e in ENGS}
        dsems = {}
        for e in ENGS:
            if self.ndma[e]:
                dsems[e] = [stack.enter_context(nc.semaphore("d_%s_%d" % (e, i)))
                            for i in range(NDMA_SLOTS)]
        ccsems = [stack.enter_context(nc.semaphore("cc_%d" % i)) for i in range(self.ncc)]
        binders = {"pe": block.tensor, "act": block.scalar, "dve": block.vector,
                   "pool": block.gpsimd, "sp": block.sync}

        def make(e):
            def body(eng):
                waited = {}

                def wait(key, sem, val):
                    if waited.get(key, 0) < val:
                        eng.wait_ge(sem, val)
                        waited[key] = val

                for o in self.ops[e]:
                    comp, dmas = o.waits
                    for d in comp:
                        wait(("c", d.eng), sems[d.eng], d.seq)
                    for d in dmas:
                        if d.dma == "cc":
                            wait(("cc", d.dma_idx), ccsems[d.dma_idx], 1)
                            continue
                        slot = d.dma_idx % NDMA_SLOTS
                        wait(("d", d.eng, slot), dsems[d.eng][slot], 16 * (d.dma_idx // NDMA_SLOTS + 1))
                    if o.dma == "cc":
                        ins = o.fn(eng)
                        ins.then_inc(ccsems[o.dma_idx], 1)
                        wait(("cc", o.dma_idx), ccsems[o.dma_idx], 1)
                    elif o.dma:
                        slot = o.dma_idx % NDMA_SLOTS
                        if o.dma_idx >= NDMA_SLOTS:
                            wait(("d", e, slot), dsems[e][slot], 16 * (o.dma_idx // NDMA_SLOTS))
                        ins = o.fn(eng)
                        ins.then_inc(dsems[e][slot], 16 if o.dma != "cc" else 16)
                    else:
                        ins = o.fn(eng)
                        if o.sig:
                            ins.then_inc(sems[e], 1)
                n = self.ndma[e]
                for slot in range(min(n, NDMA_SLOTS)):
                    cnt = (n - 1 - slot) // NDMA_SLOTS + 1
                    wait(("d", e, slot), dsems[e][slot], 16 * cnt)
            return body

        for e in ENGS:
            if self.ops[e]:
                binders[e](make(e))


def _cvec_layout():
    cols = {}
    n = 0

    def add(name, w):
        nonlocal n
        cols[name] = n
        n += w
    for l in range(2):
        add("g_mix%d" % l, 8)
        add("g_ffn%d" % l, 8)
        add("g_ple%d" % l, 8)
        add("b_q%d" % l, 4)
        add("b_k%d" % l, 4)
        add("b_xr%d" % l, 4)
        add("b_yr%d" % l, 4)
        add("cw_qk%d" % l, 32)
        add("cb_qk%d" % l, 8)
        add("cw_r%d" % l, 16)
        add("cb_r%d" % l, 4)
        add("b_ra%d" % l, 4)
        add("b_ri%d" % l, 4)
        add("lam%d" % l, 4)
        add("g_r%d" % l, 4)
    add("g_final", 8)
    add("eps", 1)
    add("one", 1)
    add("sel", 8)
    return cols, n


CV, NCV = _cvec_layout()
CR = {}
_n = 0
for _l in range(2):
    CR["b_tok%d" % _l] = _n
    _n += 1032
    CR["g_mh%d" % _l] = _n
    _n += 512
NCR = _n


def _col(v):
    return np.ascontiguousarray(np.asarray(v, np.float32).reshape(-1, 128).T)


def pack_consts(inp, core):
    cv = np.zeros((128, NCV), np.float32)

    def put(name, arr):
        arr = np.asarray(arr, np.float32)
        cv[:, CV[name]:CV[name] + arr.shape[1]] = arr
    for l in range(2):
        put("g_mix%d" % l, _col(inp["g_mix"][l]))
        put("g_ffn%d" % l, _col(inp["g_ffn"][l]))
        put("g_ple%d" % l, _col(inp["g_ple"][l]))
        b = inp["b_in"][l]
        put("b_q%d" % l, _col(b[0:512]))
        put("b_k%d" % l, _col(b[512:1024]))
        put("b_xr%d" % l, _col(b[2056:2568]))
        put("b_yr%d" % l, _col(b[2568:3080]))
        put("cw_qk%d" % l, np.concatenate([_col(inp["w_conv_qk"][l][j]) for j in range(4)], axis=1))
        put("cb_qk%d" % l, _col(inp["b_conv_qk"][l]))
        put("cw_r%d" % l, np.concatenate([_col(inp["w_conv_r"][l][j]) for j in range(4)], axis=1))
        put("cb_r%d" % l, _col(inp["b_conv_r"][l]))
        put("b_ra%d" % l, _col(inp["b_ra"][l]))
        put("b_ri%d" % l, _col(inp["b_ri"][l]))
        put("lam%d" % l, _col(inp["lam"][l]))
        put("g_r%d" % l, _col(inp["g_r"][l]))
    put("g_final", _col(inp["g_final"]))
    cv[:, CV["eps"]] = EPS
    cv[:, CV["one"]] = 1.0
    if core % 2 == 1:
        cv[:, CV["sel"] + (0 if PAIRS else core - 1)] = 1.0
    cr = np.zeros((128, NCR), np.float32)
    for l in range(2):
        cr[:, CR["b_tok%d" % l]:CR["b_tok%d" % l] + 1032] = np.asarray(inp["b_in"][l][1024:2056], np.float32)[None, :]
        cr[:, CR["g_mh%d" % l]:CR["g_mh%d" % l] + 512] = np.asarray(inp["g_mh"][l], np.float32)[None, :]
    return cv, cr


def const_mats():
    idx = np.arange(128)
    ident = np.eye(128, dtype=np.float32)
    tri = (idx[:, None] <= idx[None, :]).astype(np.float32)
    bo = ((idx[:, None] // 64) == (idx[None, :] // 64)).astype(np.float32)
    ones = np.ones((128, 128), np.float32)
    return np.concatenate([ident, tri, bo, ones], axis=1)


class Builder:
    def __init__(self, cfg):
        self.cfg = cfg
        self.nc = bass.Bass("TRN2", target_bir_lowering=False)
        self.S = Sched()
        self._mixer_w_off = 6400
        self._mixer_w_loaded = {}
        self.stack = contextlib.ExitStack()
        self.dram = {}
        self._uid = 0

    DSHAPES = {
        "x": [T, D], "p": [2, T, DPLE], "w_in": [2, D, DIN], "w_out": [2, D, D],
        "w_ff_gate": [D, DFF], "w_ff_up": [D, DFF], "w_ff_down": [DFF, D], "w_router": [D, NE],
        "w_e_gate": [NE, D, DFF], "w_e_up": [NE, D, DFF], "w_e_down": [NE, DFF, D],
        "w_ple_gate": [2, D, D], "w_ple_proj": [2, DPLE, D], "w_ra": [2, 8, 64, 64], "w_ri": [2, 8, 64, 64],
        "cvec": [128, NCV], "crow": [128, NCR], "cmat": [128, 512],
    }

    def g(self, name):
        if name not in self.dram:
            self.din(name, self.DSHAPES[name])
        return self.dram[name]

    def din(self, name, shape, dt=F32):
        t = self.nc.dram_tensor(name, list(shape), dt, kind="ExternalInput").ap()
        self.dram[name] = t
        return t

    def dout(self, name, shape, dt=F32):
        t = self.nc.dram_tensor(name, list(shape), dt, kind="ExternalOutput").ap()
        self.dram[name] = t
        return t

    def dscratch(self, name, shape, dt=F32):
        return self.nc.dram_tensor(name, list(shape), dt).ap()

    def sb(self, name, shape, dt=F32):
        return self.stack.enter_context(self.nc.sbuf_tensor(name, list(shape), dt))

    def psum(self, name, shape, dt=F32):
        return self.stack.enter_context(self.nc.psum_tensor(name, list(shape), dt))

    @staticmethod
    def fsz(ap):
        n = 1
        for d in ap.shape[1:]:
            n *= int(d)
        return n

    def op(self, eng, fn, R=(), W=()):
        return self.S.op(eng, fn, R, W)

    def dma(self, q, out, in_, R=(), W=()):
        nbytes = 128 * self.fsz(out) * 4
        return self.S.op(q, lambda e: e.dma_start(out=out, in_=in_), R, W, dma=True, cost=2500 + nbytes / 150.0)

    def mm(self, out, lhsT, rhs, start, stop, R=(), W=()):
        n = self.fsz(rhs)
        c = 64 + 0.45 * n
        if rhs.dtype == F32:
            c = 100 + 1.8 * n
        return self.S.op("pe", lambda e: e.matmul(out, lhsT, rhs, start=start, stop=stop), R, W, cost=c)

    def tr(self, out, in_, ident, R=(), W=()):
        return self.S.op("pe", lambda e: e.transpose(out, in_, ident), R, W, cost=150)

    def act(self, out, in_, func, R=(), W=(), bias=None, scale=None, accum_out=None):
        kw = {}
        if bias is not None:
            kw["bias"] = bias
        if scale is not None:
            kw["scale"] = scale
        if accum_out is not None:
            kw["accum_out"] = accum_out
        o = self.S.op("act", lambda e: e.activation(out, in_, func, **kw), R, W, cost=260 + 0.75 * self.fsz(out))
        if func in (AF.Exp, AF.Ln, AF.Sigmoid, AF.Silu, AF.Sqrt):
            o.table = func
        return o

    def tt(self, eng, out, in0, in1, op, R=(), W=()):
        c = (80 + 1.05 * self.fsz(out)) if eng == "dve" else (200 + 2.1 * self.fsz(out))
        return self.S.op(eng, lambda e: e.tensor_tensor(out, in0, in1, op), R, W, cost=c)

    def ts(self, eng, out, in0, s1, s2, op0, op1=None, R=(), W=()):
        c = (80 + 1.05 * self.fsz(out)) if eng == "dve" else (200 + 2.1 * self.fsz(out))
        if op1 is None:
            return self.S.op(eng, lambda e: e.tensor_scalar(out, in0, s1, None, op0), R, W, cost=c)
        return self.S.op(eng, lambda e: e.tensor_scalar(out, in0, s1, s2, op0, op1), R, W, cost=c)

    def stt(self, out, in0, scalar, in1, op0, op1, R=(), W=()):
        return self.S.op("dve", lambda e: e.scalar_tensor_tensor(out, in0, scalar, in1, op0, op1), R, W,
                         cost=80 + 1.6 * self.fsz(out))

    def copy(self, eng, out, in_, R=(), W=()):
        if eng == "act":
            return self.S.op("act", lambda e: e.copy(out, in_), R, W, cost=260 + 0.75 * self.fsz(out))
        c = (80 + 1.05 * self.fsz(out)) if eng == "dve" else (200 + 2.1 * self.fsz(out))
        return self.S.op(eng, lambda e: e.tensor_copy(out, in_), R, W, cost=c)

    def recip(self, out, in_, R=(), W=()):
        return self.S.op("dve", lambda e: e.reciprocal(out, in_), R, W, cost=100 + 4.0 * self.fsz(out))

    def setup(self):
        cfg = self.cfg
        if cfg.get("mode") != "A":
            self.out = self.dout("out", [T, D])

        sb = self.sb
        self.hT = sb("hT", [128, KC * T], F32)
        self.hT3 = self.hT[:].rearrange("p (c t) -> p c t", c=KC)
        self.cvec = sb("cvec_s", [128, NCV], F32)
        self.cmat = sb("cmat_s", [128, 512], F32)
        self.ident_f = self.cmat[:, 0:128]
        self.tri_f = self.cmat[:, 128:256]
        self.bo_f = self.cmat[:, 256:384]
        self.ones_f = self.cmat[:, 384:512]
        self.ident_b = sb("ident_b", [128, 128], BF16)
        self.ones_b = sb("ones_b", [128, 128], BF16)
        self.ps = [self.psum("ps%d" % i, [128, 512], F32) for i in range(8)]
        self.sq = [sb("sq%d" % i, [128, TB], BF16) for i in range(2)]
        self.rs = sb("rs", [128, TB], F32)
        BIGW = cfg.get("big_words", 34 * 1024)
        self.BIGW = BIGW
        self.big = sb("big", [128, BIGW], F32)
        self.xin = [self.big[:, BIGW - 4096 + i * 1024:BIGW - 4096 + (i + 1) * 1024] for i in range(2)]
        self.hn3 = self.big[:, BIGW - 2048:BIGW].bitcast(BF16).rearrange("p (c t) -> p c t", c=KC)

        self.dma("sp", self.cvec[:], self.g("cvec")[:, :], W=["cvec"])
        self.dma("sp", self.cmat[:], self.g("cmat")[:, :], W=["cmat"])
        self.copy("dve", self.ident_b[:], self.ident_f, R=["cmat"], W=["ident_b"])
        self.copy("dve", self.ones_b[:], self.ones_f, R=["cmat"], W=["ones_b"])

    def cv(self, name, j=0, w=1):
        c = CV[name] + j
        return self.cvec[:, c:c + w]

    def hres(self, t0, t1):
        return [("hT", i) for i in range(t0 // 128, (t1 + 127) // 128)]

    def load_x(self):
        for tt in range(T // 128):
            xb = self.xin[tt % 2]
            r = ("xin", tt % 2)
            self.dma("sp", xb, self.g("x")[tt * 128:(tt + 1) * 128, :], W=[r])
            for half in range(2):
                bank = 6 + half
                for j in range(4):
                    c = half * 4 + j
                    self.tr(self.ps[bank][:, j * 128:(j + 1) * 128], xb[:, c * 128:(c + 1) * 128],
                            self.ident_f, R=[r, "cmat"], W=[("ps", bank)])
                eng = "dve" if half == 0 else "act"
                self.copy(eng, self.hT3[:, half * 4:half * 4 + 4, tt * 128:(tt + 1) * 128],
                          self.ps[bank][:].rearrange("p (c t) -> p c t", c=4),
                          R=[("ps", bank)], W=[("hT", tt)])

    def store_out(self):
        for tt in range(T // 128):
            xb = self.xin[tt % 2]
            r = ("xin", tt % 2)
            for half in range(2):
                bank = 6 + half
                for j in range(4):
                    c = half * 4 + j
                    self.tr(self.ps[bank][:, j * 128:(j + 1) * 128], self.hT3[:, c, tt * 128:(tt + 1) * 128],
                            self.ident_f, R=[("hT", tt), "cmat"], W=[("ps", bank)])
                eng = "dve" if half == 0 else "act"
                self.copy(eng, xb[:, half * 512:(half + 1) * 512], self.ps[bank][:],
                          R=[("ps", bank)], W=[r])
            self.dma("sp", self.out[tt * 128:(tt + 1) * 128, :], xb, R=[r], W=[("out", tt)])

    def norm_stats(self, t0, n, bank=6):
        hr = self.hres(t0, t0 + n)
        for c in range(KC):
            sq = self.sq[c % 2]
            self.act(sq[:, :n], self.hT3[:, c, t0:t0 + n], AF.Square, R=hr, W=[("sq", c % 2)])
            self.mm(self.ps[bank][:, :n], self.ones_b[:], sq[:, :n], c == 0, c == KC - 1,
                    R=[("sq", c % 2), "ones_b"], W=[("ps", bank)])
        self.act(self.rs[:, :n], self.ps[bank][:, :n], AF.Sqrt, R=[("ps", bank), "cvec"], W=["rs"],
                 bias=self.cv("eps"), scale=1.0 / D)
        self.recip(self.rs[:, :n], self.rs[:, :n], R=["rs"], W=["rs"])

    def norm_apply(self, t0, n, gname, dst3, dres):
        hr = self.hres(t0, t0 + n)
        for c in range(KC):
            self.stt(dst3[:, c, :n], self.hT3[:, c, t0:t0 + n], self.cv(gname, c), self.rs[:, :n],
                     ALU.mult, ALU.mult, R=hr + ["rs", "cvec"], W=[dres])

    def final_norm(self):
        dbg = self.cfg.get("dbg")
        if dbg is not None:
            hr = self.hres(0, TB)
            if dbg == "act":
                self.act(self.sq[0][:, :TB], self.hT3[:, 0, 0:TB], AF.Square, R=hr, W=[("sq", 0)])
            elif dbg == "dve":
                self.ts("dve", self.hT3[:, 0, 0:TB], self.hT3[:, 0, 0:TB], 1.0, None, ALU.mult, R=hr, W=hr)
            elif dbg == "dve2":
                self.ts("dve", self.rs[:, 0:TB], self.hT3[:, 0, 0:TB], 1.0, None, ALU.mult, R=hr, W=["rs"])
            elif dbg == "dve3":
                self.ts("dve", self.rs[:, 0:TB], self.rs[:, 0:TB], 1.0, None, ALU.mult, R=[], W=["rs"])
            elif dbg == "dve4":
                self.copy("dve", self.rs[:, 0:TB], self.rs[:, 0:TB], R=[], W=["rs"])
            elif dbg == "dve5":
                sv = self.S.bar
                self.S.bar = []
                self.ts("dve", self.rs[:, 0:TB], self.rs[:, 0:TB], 1.0, None, ALU.mult, R=[], W=["rs"])
                self.S.bar = sv
            elif dbg == "dve6":
                self.stt(self.rs[:, 0:TB], self.rs[:, 0:TB], 1.0, self.rs[:, 0:TB], ALU.mult, ALU.mult, R=[], W=["rs"])
            elif dbg.startswith("dveN"):
                for i in range(int(dbg[4:])):
                    self.copy("dve", self.rs[:, 0:TB], self.rs[:, 0:TB], R=[], W=["rs"])
            elif dbg.startswith("actN"):
                for i in range(int(dbg[4:])):
                    self.act(self.sq[0][:, :TB], self.hT3[:, 0, 0:TB], AF.Square, R=hr, W=[("sq", 0)])
            elif dbg == "pe":
                self.mm(self.ps[6][:, :TB], self.ones_b[:], self.ones_b[:], True, True, R=["ones_b"], W=[("ps", 6)])
            elif dbg == "pool":
                self.ts("pool", self.hT3[:, 0, 0:TB], self.hT3[:, 0, 0:TB], 1.0, None, ALU.mult, R=hr, W=hr)
            return
        for tb in range(NTB):
            t0 = tb * TB
            self.norm_stats(t0, TB)
            hr = self.hres(t0, t0 + TB)
            for c in range(KC):
                self.stt(self.hT3[:, c, t0:t0 + TB], self.hT3[:, c, t0:t0 + TB], self.cv("g_final", c),
                         self.rs[:, :TB], ALU.mult, ALU.mult, R=hr + ["rs", "cvec"], W=hr)

    def load_w(self, dst, src, res):
        return self.dma("pool", dst, src, W=[res])

    def ple(self, l):
        big = self.big
        o = self._mixer_w_off + KC * DIN // 2 + KC * D // 2
        if l == 0 and self.cfg.get("prefetch_next", True) and ("mixer", 1) in self.cfg["phases"]:
            self.mixer_prefetch(1)
        wpg = big[:, o:o + 4096].bitcast(BF16).rearrange("p (k n) -> p k n", k=KC)
        o += 4096
        wpp = big[:, o:o + 1024].bitcast(BF16).rearrange("p (k n) -> p k n", k=2)
        o += 1024
        pst = big[:, o:o + 1024].rearrange("p (i n) -> p i n", i=4)
        o += 1024
        pT = big[:, o:o + 512].bitcast(BF16).rearrange("p (j t) -> p j t", j=2)
        o += 512
        assert o <= self.BIGW - 4096
        sg = [big[:, i * 512:(i + 1) * 512] for i in range(2)]
        tmp = [big[:, 1024 + i * 512:1024 + (i + 1) * 512] for i in range(2)]
        self.load_w(wpg, self.g("w_ple_gate")[l].rearrange("(k p) n -> p k n", p=128), "wpg")
        self.load_w(wpp, self.g("w_ple_proj")[l].rearrange("(k p) n -> p k n", p=128), "wpp")
        for tb in range(NTB):
            t0 = tb * TB
            hr = self.hres(t0, t0 + TB)
            self.dma("sp", pst, self.g("p")[l, t0:t0 + TB, :].rearrange("(i q) n -> q i n", q=128), W=["pst"])
            self.norm_stats(t0, TB)
            self.norm_apply(t0, TB, "g_ple%d" % l, self.hn3, "hn")
            for j in range(2):
                for i in range(4):
                    self.tr(self.ps[7][:, i * 128:(i + 1) * 128], pst[:, i, j * 128:(j + 1) * 128],
                            self.ident_f, R=["pst", "cmat"], W=[("ps", 7)])
                self.copy("act", pT[:, j, :], self.ps[7][:], R=[("ps", 7)], W=["pT"])
            for fo in range(KC):
                a = fo % 2
                b = 2 + fo % 2
                for kc in range(KC):
                    self.mm(self.ps[a][:], wpg[:, kc, fo * 128:(fo + 1) * 128], self.hn3[:, kc, :],
                            kc == 0, kc == KC - 1, R=["wpg", "hn"], W=[("ps", a)])
                for j in range(2):
                    self.mm(self.ps[b][:], wpp[:, j, fo * 128:(fo + 1) * 128], pT[:, j, :],
                            j == 0, j == 1, R=["wpp", "pT"], W=[("ps", b)])
                self.act(sg[fo % 2], self.ps[a][:], AF.Sigmoid, R=[("ps", a)], W=[("sg", fo % 2)])
                self.tt("dve", tmp[fo % 2], self.ps[b][:], sg[fo % 2], ALU.mult,
                        R=[("ps", b), ("sg", fo % 2)], W=[("ptmp", fo % 2)])
                self.tt("pool", self.hT3[:, fo, t0:t0 + TB], self.hT3[:, fo, t0:t0 + TB], tmp[fo % 2], ALU.add,
                        R=hr + [("ptmp", fo % 2)], W=hr)

    def ffn_norm_all(self, gname, hn_all3):
        for tb in range(NTB):
            t0 = tb * TB
            self.norm_stats(t0, TB)
            hr = self.hres(t0, t0 + TB)
            for c in range(KC):
                self.stt(hn_all3[:, c, t0:t0 + TB], self.hT3[:, c, t0:t0 + TB], self.cv(gname, c),
                         self.rs[:, :TB], ALU.mult, ALU.mult, R=hr + ["rs", "cvec"], W=[("hna", tb)])
            if self.cfg.get("moe_hook") is not None and gname.startswith("g_ffn1"):
                self.cfg["moe_hook"](tb)

    def ffn_core(self, experts, hn_all3, o, gate_fn=None):
        big = self.big
        GS = [(0, 4), (4, 4), (8, 4), (12, 4), (16, 4), (20, 2)]
        wgb, wub, wdb = [], [], []
        for i in range(2):
            wgb.append(big[:, o:o + 2048].bitcast(BF16).rearrange("p (k n) -> p k n", k=KC))
            o += 2048
            wub.append(big[:, o:o + 2048].bitcast(BF16).rearrange("p (k n) -> p k n", k=KC))
            o += 2048
            wdb.append(big[:, o:o + 2048].bitcast(BF16).rearrange("p (j n) -> p j n", j=4))
            o += 2048
        actb = []
        for i in range(2):
            actb.append(big[:, o:o + 1024].bitcast(BF16).rearrange("p (j t) -> p j t", j=4))
            o += 1024
        st = [big[:, o + i * 512:o + (i + 1) * 512] for i in range(2)]
        o += 1024
        st2 = [big[:, o + i * 256:o + (i + 1) * 256].bitcast(BF16) for i in range(2)]
        o += 512
        self._ffn_end = o

        steps = []
        gi = 0
        for e, (wg, wu, wd) in enumerate(experts):
            for (c0, nch) in GS:
                for tb in range(NTB):
                    steps.append((e, gi, c0, nch, tb))
                gi += 1

        def load_group(e, g, c0, nch):
            wg, wu, wd = experts[e]
            par = g % 2
            n = nch * 128
            self.load_w(wgb[par][:, :, :n], wg[:, c0 * 128:c0 * 128 + n].rearrange("(k p) n -> p k n", p=128),
                        ("wgb", par))
            self.load_w(wub[par][:, :, :n], wu[:, c0 * 128:c0 * 128 + n].rearrange("(k p) n -> p k n", p=128),
                        ("wub", par))
            self.load_w(wdb[par][:, :nch, :], wd[c0 * 128:c0 * 128 + n, :].rearrange("(j p) f -> p j f", p=128),
                        ("wdb", par))

        groups = []
        for e in range(len(experts)):
            for (c0, nch) in GS:
                groups.append((e, len(groups), c0, nch))
        load_group(*groups[0])

        def down(step, sidx):
            e, g, c0, nch, tb = step
            par = g % 2
            t0 = tb * TB
            hr = self.hres(t0, t0 + TB)
            ab = actb[sidx % 2]
            for fo in range(KC):
                bank = 4 + fo % 2
                for j in range(nch):
                    self.mm(self.ps[bank][:], wdb[par][:, j, fo * 128:(fo + 1) * 128], ab[:, j, :],
                            j == 0, j == nch - 1, R=[("wdb", par), ("actb", sidx % 2)], W=[("ps", bank)])
                self.tt("dve", self.hT3[:, fo, t0:t0 + TB], self.hT3[:, fo, t0:t0 + TB], self.ps[bank][:], ALU.add,
                        R=hr + [("ps", bank)], W=hr)

        prev = None
        for sidx, step in enumerate(steps):
            e, g, c0, nch, tb = step
            par = g % 2
            t0 = tb * TB
            ab = actb[sidx % 2]
            gate = gate_fn(e) if gate_fn is not None else None
            if gate_fn is not None and c0 == 4 and tb == 0:
                gate_fn(e + 1)
            for j in range(nch):
                a = j % 2
                b = 2 + j % 2
                for kc in range(KC):
                    self.mm(self.ps[a][:], wgb[par][:, kc, j * 128:(j + 1) * 128], hn_all3[:, kc, t0:t0 + TB],
                            kc == 0, kc == KC - 1, R=[("wgb", par), ("hna", tb)], W=[("ps", a)])
                for kc in range(KC):
                    self.mm(self.ps[b][:], wub[par][:, kc, j * 128:(j + 1) * 128], hn_all3[:, kc, t0:t0 + TB],
                            kc == 0, kc == KC - 1, R=[("wub", par), ("hna", tb)], W=[("ps", b)])
                if gate is None:
                    self.act(st2[j % 2], self.ps[a][:], AF.Silu, R=[("ps", a)], W=[("st2", j % 2)])
                else:
                    gap, gres = gate
                    self.act(st[j % 2], self.ps[a][:], AF.Silu, R=[("ps", a)], W=[("st", j % 2)])
                    self.tt("pool", st2[j % 2], st[j % 2], gap[:, t0:t0 + TB], ALU.mult,
                            R=[("st", j % 2), gres], W=[("st2", j % 2)])
                self.tt("dve", ab[:, j, :], self.ps[b][:], st2[j % 2], ALU.mult,
                        R=[("ps", b), ("st2", j % 2)], W=[("actb", sidx % 2)])
            if prev is not None:
                down(*prev)
            prev = (step, sidx)
            if tb == 0 and g + 1 < len(groups):
                load_group(*groups[g + 1])
        down(*prev)

    def ffn_dense(self):
        big = self.big
        hn_all3 = big[:, 0:8192].bitcast(BF16).rearrange("p (c t) -> p c t", c=KC)
        self.ffn_norm_all("g_ffn0", hn_all3)
        self.ffn_core([(self.g("w_ff_gate"), self.g("w_ff_up"), self.g("w_ff_down"))], hn_all3, 8192)


    def moe(self):
        big = self.big
        hn_all3 = big[:, 0:8192].bitcast(BF16).rearrange("p (c t) -> p c t", c=KC)
        o = 8192
        wr = big[:, o:o + 64].rearrange("p (k e) -> p k e", k=KC)
        o += 64
        gates = big[:, o:o + 128].rearrange("p (t e) -> p t e", t=16)
        o += 128
        sm = big[:, o:o + 64]
        o += 64
        diag = [big[:, o + i * 512:o + (i + 1) * 512] for i in range(2)]
        o += 1024
        grep = [big[:, o + i * 1024:o + (i + 1) * 1024].bitcast(BF16) for i in range(2)]
        o += 2048
        lgt, eq1, lg2, eq2 = sm[:, 0:9], sm[:, 16:24], sm[:, 24:32], sm[:, 32:40]
        m1, m2, dd, g1, g2 = sm[:, 40:41], sm[:, 41:42], sm[:, 42:43], sm[:, 43:44], sm[:, 44:45]

        self.dma("sp", wr, self.g("w_router").rearrange("(k p) e -> p k e", p=128), W=["wr"])
        for k in range(KC):
            self.ts("dve", wr[:, k, :], wr[:, k, :], self.cv("g_ffn1", k), None, ALU.mult,
                    R=["wr", "cvec"], W=["wr"])

        def route(tb):
            for i in range(4):
                tt = tb * 4 + i
                hr = [("hT", tt)]
                for kc in range(KC):
                    self.mm(self.ps[7][:, 0:8], self.hT3[:, kc, tt * 128:(tt + 1) * 128], wr[:, kc, :],
                            kc == 0, kc == KC - 1, R=hr + ["wr"], W=[("ps", 7)])
                self.mm(self.ps[7][:, 8:9], self.rs[0:1, i * 128:(i + 1) * 128], self.ones_f[0:1, 0:1],
                        True, True, R=["rs", "cmat"], W=[("ps", 7)])
                self.copy("dve", lgt, self.ps[7][:, 0:9], R=[("ps", 7)], W=["sm"])
                S = self.S
                S.op("dve", lambda e: e.reduce_max(m1, lgt[:, 0:8], AX.X), ["sm"], ["sm"])
                self.ts("dve", eq1, lgt[:, 0:8], m1, None, ALU.is_equal, R=["sm"], W=["sm"])
                self.stt(lg2, eq1, -1e30, lgt[:, 0:8], ALU.mult, ALU.add, R=["sm"], W=["sm"])
                S.op("dve", lambda e: e.reduce_max(m2, lg2, AX.X), ["sm"], ["sm"])
                self.ts("dve", eq2, lg2, m2, None, ALU.is_equal, R=["sm"], W=["sm"])
                self.ts("dve", dd, m1, m2, lgt[:, 8:9], ALU.subtract, ALU.mult, R=["sm"], W=["sm"])
                self.act(g1, dd, AF.Sigmoid, R=["sm"], W=["sm"])
                self.ts("dve", g2, g1, -1.0, 1.0, ALU.mult, ALU.add, R=["sm"], W=["sm"])
                self.ts("dve", gates[:, tt, :], eq1, g1, None, ALU.mult, R=["sm"], W=["gates"])
                self.stt(gates[:, tt, :], eq2, g2, gates[:, tt, :], ALU.mult, ALU.add, R=["sm", "gates"], W=["gates"])

        self.cfg["moe_hook"] = route
        self.ffn_norm_all("g_ffn1", hn_all3)
        self.cfg["moe_hook"] = None

        built = {}

        def gate_fn(e):
            if e in built or e >= NE:
                return built.get(e)
            gp = grep[e % 2]
            res = ("grep", e % 2)
            for q in range(4):
                dg = diag[q % 2]
                for j in range(4):
                    tt = q * 4 + j
                    self.ts("pool", dg[:, j * 128:(j + 1) * 128], self.ident_f, gates[:, tt, e:e + 1], None, ALU.mult,
                            R=["gates", "cmat"], W=[("diag", q % 2)])
                self.mm(self.ps[7][:], self.ones_f, dg, True, True, R=[("diag", q % 2), "cmat"], W=[("ps", 7)])
                self.copy("act", gp[:, q * 512:(q + 1) * 512], self.ps[7][:], R=[("ps", 7)], W=[res])
            built[e] = (gp, res)
            return built[e]

        wg, wu, wd = self.g("w_e_gate"), self.g("w_e_up"), self.g("w_e_down")
        nexp = self.cfg.get("n_experts", NE)
        self.ffn_core([(wg[e], wu[e], wd[e]) for e in range(nexp)], hn_all3, o, gate_fn)


    def exchange(self, l, tag, src_ap, W_, dst_ap):
        NR = 2 if self.cfg.get("pairs", True) else NCORES
        groups = [[2 * i, 2 * i + 1] for i in range(NCORES // 2)] if self.cfg.get("pairs", True) else [list(range(NCORES))]
        ccs = self.dscratch("ccs_%s%d" % (tag, l), [128, W_])
        ccd = self.dscratch("ccd_%s%d" % (tag, l), [NR * 128, W_])
        gath = self.big[:, 0:NR * W_].rearrange("p (r w) -> p r w", r=NR)
        self.S.barrier()
        if self.cfg.get("no_cc"):
            self.ts("dve", dst_ap, src_ap, 0.0, None, ALU.mult, R=["xsrc"], W=["xdst"])
            self.S.barrier()
            return
        self.dma("pool", ccs[:, :], src_ap, R=["xsrc"], W=["ccs"])
        if not self.cfg.get("skip_cc"):
            self.S.op("pool", lambda e: e.collective_compute("AllGather", ALU.bypass,
                                                             replica_groups=groups,
                                                             ins=[ccs[:, :]], outs=[ccd[:, :]]),
                      R=["ccs"], W=["ccd"], dma="cc")
        qg = self.cfg.get("gq", "pool")
        self.dma(qg, gath, ccd.rearrange("(r p) w -> p r w", r=NR), R=["ccd"], W=["gath"])
        self.ts("dve", dst_ap, gath[:, 0, :], self.cv("sel", 0), None, ALU.mult, R=["gath", "cvec"], W=["xdst"])
        for r in range(1, NR):
            self.stt(dst_ap, gath[:, r, :], self.cv("sel", r), dst_ap, ALU.mult, ALU.add,
                     R=["gath", "cvec", "xdst"], W=["xdst"])
        self.S.barrier()

    def mixer_prefetch(self, l):
        big = self.big
        o0 = self._mixer_w_off
        win = big[:, o0:o0 + KC * DIN // 2].bitcast(BF16).rearrange("p (k n) -> p k n", k=KC)
        o1 = o0 + KC * DIN // 2
        wout = big[:, o1:o1 + KC * D // 2].bitcast(BF16).rearrange("p (k n) -> p k n", k=KC)
        self.load_w(win, self.g("w_in")[l].rearrange("(k p) n -> p k n", p=128), "win")
        self.load_w(wout, self.g("w_out")[l].rearrange("(k p) n -> p k n", p=128), "wout")
        self._mixer_w_loaded[l] = True

    def mixer(self, l):
        TM = 256
        big = self.big
        off = [0]

        def take(words):
            a = big[:, off[0]:off[0] + words]
            off[0] += words
            return a
        rg_sets = [[take(TM) for _ in range(8)] for _ in range(2)]
        P_t = take(512)
        erow = take(256).bitcast(BF16)
        Sw = take(256).bitcast(BF16)
        qs = take(256).bitcast(BF16)
        kw = take(256).bitcast(BF16)
        hmo = take(512)
        trilf = hmo
        tmpo = hmo
        hmn = take(256).bitcast(BF16)
        assert off[0] >= NCORES * 520
        xpack = big[:, 2 * 556:3 * 556]
        xrecv = big[:, 3 * 556:4 * 556]
        assert off[0] == self._mixer_w_off, (off[0], self._mixer_w_off)
        win = take(KC * DIN // 2).bitcast(BF16).rearrange("p (k n) -> p k n", k=KC)
        wout = take(KC * D // 2).bitcast(BF16).rearrange("p (k n) -> p k n", k=KC)
        bda = take(512).rearrange("p (c n) -> p c n", c=4)
        bdi = take(512).rearrange("p (c n) -> p c n", c=4)
        crow = take(1544)
        b_tok = crow[:, 0:1032]
        g_mh = crow[:, 1032:1544]
        halo0 = take(36).rearrange("p (j n) -> p j n", j=12)
        halo = take(36).rearrange("p (j n) -> p j n", j=12)
        tailb = take(36).rearrange("p (j n) -> p j n", j=12)
        state = take(520)
        Caug = state[:, 0:516].rearrange("p (h n) -> p h n", h=4)
        rstate = state[:, 516:520]
        state_in = take(520)
        Cbf = take(258).bitcast(BF16).rearrange("p (h n) -> p h n", h=4)
        kap = take(4)
        kap2 = take(4)
        smx = take(48)
        qT = take(4 * TM // 2).bitcast(BF16).rearrange("p (h t) -> p h t", h=4)
        kT = take(4 * TM // 2).bitcast(BF16).rearrange("p (h t) -> p h t", h=4)
        hmT = take(4 * TM // 2).bitcast(BF16).rearrange("p (h t) -> p h t", h=4)
        hrn = take(4 * TM // 2).bitcast(BF16).rearrange("p (h t) -> p h t", h=4)
        zraw = [take(TM + 4) for _ in range(2)]
        accb = [take(TM) for _ in range(2)]
        vaug = [take(258).bitcast(BF16).rearrange("p (h n) -> p h n", h=4) for _ in range(2)]
        osig = take(256).bitcast(BF16)
        ifg = take(8)
        assert off[0] <= self.BIGW - 4096, (off[0], self.BIGW)
        hn3 = self.hn3
        ps = self.ps
        bctr = [0]

        def nb():
            b = bctr[0] % 8
            bctr[0] += 1
            return b
        L = str(l)
        w_in = self.g("w_in")[l]

        def cvl(name, j=0, w=1):
            return self.cv(name + L, j, w)

        if not self._mixer_w_loaded.get(l):
            self.mixer_prefetch(l)
        self.dma("sp", crow, self.g("crow")[:, l * 1544:(l + 1) * 1544], W=["crow"])
        self.op("pool", lambda e: e.memset(bda, 0.0), W=["bda"])
        self.op("pool", lambda e: e.memset(bdi, 0.0), W=["bdi"])
        for par in range(2):
            lo = par * 64
            self.dma("sp", bda[lo:lo + 64, :, lo:lo + 64],
                     self.g("w_ra")[l].rearrange("(c two) d e -> two d c e", two=2)[par], R=[], W=["bda"])
            self.dma("sp", bdi[lo:lo + 64, :, lo:lo + 64],
                     self.g("w_ri")[l].rearrange("(c two) d e -> two d c e", two=2)[par], R=[], W=["bdi"])
        for i in range(2):
            self.op("pool", lambda e, i=i: e.memset(vaug[i][:, :, 128:129], 1.0), W=[("vaug", i)])
        self.act(kap, cvl("lam", 0, 4), AF.Exp, R=["cvec"], W=["kap"], scale=-1.0)
        self.act(kap, kap, AF.Ln, R=["kap", "cvec"], W=["kap"], bias=self.cv("one"))
        self.ts("dve", kap2, kap, -16.0, None, ALU.mult, R=["kap"], W=["kap2"])
        self.ts("dve", kap, kap, -8.0, None, ALU.mult, R=["kap"], W=["kap"])

        def fm_desc(kind, c):
            if kind == "q":
                return (c, c * 128, cvl("b_q", c), [cvl("cw_qk", t * 8 + c) for t in range(4)], cvl("cb_qk", c))
            if kind == "k":
                return (4 + c, 512 + c * 128, cvl("b_k", c), [cvl("cw_qk", t * 8 + 4 + c) for t in range(4)],
                        cvl("cb_qk", 4 + c))
            return (8 + c, 2056 + c * 128, cvl("b_xr", c), [cvl("cw_r", t * 4 + c) for t in range(4)], cvl("cb_r", c))

        mode = self.cfg.get("mode", "F")
        if mode != "B":
            self.norm_stats(T - 128, 128, bank=nb())
            self.norm_apply(T - 128, 128, "g_mix" + L, hn3, "hn")
            n = 0
            for kind in ("q", "k", "x"):
                for c in range(4):
                    jdx, co, bias, taps, cb = fm_desc(kind, c)
                    bank = nb()
                    n += 1
                    for kc in range(KC):
                        self.mm(ps[bank][:, :128], win[:, kc, co:co + 128], hn3[:, kc, :128], kc == 0, kc == KC - 1,
                                R=["win", "hn"], W=[("ps", bank)])
                    self.act(tailb[:, jdx, :], ps[bank][:, 125:128], AF.Identity, R=[("ps", bank), "cvec"], W=["xsrc"],
                             bias=bias)
        if mode == "F":
            self.op("pool", lambda e: e.memset(halo0.rearrange("p j n -> p (j n)"), 0.0), W=["xdst"])
        elif mode == "A":
            o_tail = self.dout("o_tail", [128, 36])
            self.dma("sp", o_tail[:, :], tailb.rearrange("p j n -> p (j n)"), R=["xsrc"], W=["o_tail"])
            self.op("pool", lambda e: e.memset(halo0.rearrange("p j n -> p (j n)"), 0.0), W=["xdst"])
        else:
            i_tail = self.din("i_tail", [128, 36])
            self.dma("sp", halo0.rearrange("p j n -> p (j n)"), i_tail[:, :], W=["xdst"])

        cnt = {"fm": 0}

        def conv_chunk(kind, c, t0):
            jdx, co, bias, taps, cb = fm_desc(kind, c)
            rot = cnt["fm"] % 2
            cnt["fm"] += 1
            bank = nb()
            for kc in range(KC):
                self.mm(ps[bank][:, :TM], win[:, kc, co:co + 128], hn3[:, kc, :TM], kc == 0, kc == KC - 1,
                        R=["win", "hn"], W=[("ps", bank)])
            zr = zraw[rot]
            zres = ("zraw", rot)
            hres_ = ("halo", jdx)
            self.copy("pool", zr[:, 0:3], halo[:, jdx, :], R=[hres_], W=[zres])
            self.act(zr[:, 3:3 + TM], ps[bank][:, :TM], AF.Identity, R=[("ps", bank), "cvec"], W=[zres], bias=bias)
            self.copy("pool", halo[:, jdx, :], zr[:, TM:TM + 3], R=[zres], W=[hres_])
            acc = accb[rot]
            ares = ("acc", rot)
            self.ts("dve", acc, zr[:, 3:3 + TM], taps[3], cb, ALU.mult, ALU.add, R=[zres, "cvec"], W=[ares])
            for t in (2, 1, 0):
                self.stt(acc, zr[:, t:t + TM], taps[t], acc, ALU.mult, ALU.add, R=[zres, ares, "cvec"], W=[ares])
            return acc, ares

        def rglru_chunk(c, xr, xres, state_only, t0):
            r_t, ig_t, a_t, a2_t, u_t, hr_t, gel_t, x2_t = rg_sets[c % 2]
            sfx = "%d" % (c % 2)
            bA, bI = nb(), nb()
            self.mm(ps[bA][:, :TM], bda[:, c, :], xr, True, True, R=["bda", xres], W=[("ps", bA)])
            self.mm(ps[bI][:, :TM], bdi[:, c, :], xr, True, True, R=["bdi", xres], W=[("ps", bI)])
            self.act(r_t, ps[bA][:, :TM], AF.Sigmoid, R=[("ps", bA), "cvec"], W=["r_t" + sfx], bias=cvl("b_ra", c))
            self.act(ig_t, ps[bI][:, :TM], AF.Sigmoid, R=[("ps", bI), "cvec"], W=["ig_t" + sfx], bias=cvl("b_ri", c))
            self.act(a_t, r_t, AF.Exp, R=["r_t" + sfx, "kap"], W=["a_t" + sfx], scale=kap[:, c:c + 1])
            self.act(a2_t, r_t, AF.Exp, R=["r_t" + sfx, "kap2"], W=["a2_t" + sfx], scale=kap2[:, c:c + 1])
            self.act(a2_t, a2_t, AF.Sqrt, R=["a2_t" + sfx, "cvec"], W=["a2_t" + sfx], scale=-1.0, bias=self.cv("one"))
            self.tt("pool", ig_t, ig_t, xr, ALU.mult, R=["ig_t" + sfx, xres], W=["ig_t" + sfx])
            self.tt("dve", u_t, ig_t, a2_t, ALU.mult, R=["ig_t" + sfx, "a2_t" + sfx], W=["u_t" + sfx])
            self.S.op("dve", lambda e: e.tensor_tensor_scan(hr_t, a_t, u_t, rstate[:, c:c + 1], ALU.mult, ALU.add),
                      R=["a_t" + sfx, "u_t" + sfx, "rstate"], W=["hr_t" + sfx])
            self.copy("act", rstate[:, c:c + 1], hr_t[:, TM - 1:TM], R=["hr_t" + sfx], W=["rstate"])
            if state_only:
                return
            co = 2568 + c * 128
            bank = nb()
            for kc in range(KC):
                self.mm(ps[bank][:, :TM], win[:, kc, co:co + 128], hn3[:, kc, :TM], kc == 0, kc == KC - 1,
                        R=["win", "hn"], W=[("ps", bank)])
            self.act(gel_t, ps[bank][:, :TM], AF.Identity, R=[("ps", bank), "cvec"], W=["gel_t" + sfx], bias=cvl("b_yr", c))
            self.act(x2_t, gel_t, AF.Square, R=["gel_t" + sfx], W=["x2_t" + sfx])
            self.ts("dve", x2_t, x2_t, 0.044715, 1.0, ALU.mult, ALU.add, R=["x2_t" + sfx], W=["x2_t" + sfx])
            self.tt("dve", x2_t, x2_t, gel_t, ALU.mult, R=["x2_t" + sfx, "gel_t" + sfx], W=["x2_t" + sfx])
            self.act(x2_t, x2_t, AF.Sigmoid, R=["x2_t" + sfx], W=["x2_t" + sfx], scale=1.5957691216057308)
            self.tt("pool", gel_t, gel_t, x2_t, ALU.mult, R=["gel_t" + sfx, "x2_t" + sfx], W=["gel_t" + sfx])
            self.tt("dve", gel_t, gel_t, hr_t, ALU.mult, R=["gel_t" + sfx, "hr_t" + sfx], W=["gel_t" + sfx])
            self.act(x2_t, gel_t, AF.Square, R=["gel_t" + sfx], W=["x2_t" + sfx])
            bG = nb()
            self.mm(ps[bG][:, :TM], self.bo_f, x2_t, True, True, R=["cmat", "x2_t" + sfx], W=[("ps", bG)])
            self.act(x2_t, ps[bG][:, :TM], AF.Sqrt, R=[("ps", bG), "cvec"], W=["x2_t" + sfx], scale=1.0 / 64, bias=self.cv("eps"))
            self.recip(x2_t, x2_t, R=["x2_t" + sfx], W=["x2_t" + sfx])
            self.stt(hrn[:, c, :], gel_t, cvl("g_r", c), x2_t, ALU.mult, ALU.mult, R=["gel_t" + sfx, "x2_t" + sfx, "cvec"], W=["hrn"])

        def tok_major(i, state_only, vi):
            va = vaug[vi]
            vres = ("vaug", vi)
            lhs = lambda kc: hn3[:, kc, i * 128:(i + 1) * 128]
            bv, bif = nb(), nb()
            for kc in range(KC):
                self.mm(ps[bv][:], lhs(kc), win[:, kc, 1024:1536], kc == 0, kc == KC - 1, R=["win", "hn"], W=[("ps", bv)])
            self.tt("dve", va[:, :, 0:128], ps[bv][:].rearrange("p (h n) -> p h n", h=4),
                    b_tok[:, 0:512].rearrange("p (h n) -> p h n", h=4), ALU.add, R=[("ps", bv), "crow"], W=[vres])
            for kc in range(KC):
                self.mm(ps[bif][:, 16:24], lhs(kc), win[:, kc, 2048:2056], kc == 0, kc == KC - 1, R=["win", "hn"], W=[("ps", bif)])
            self.tt("dve", ifg, ps[bif][:, 16:24], b_tok[:, 1024:1032], ALU.add, R=[("ps", bif), "crow"], W=["ifg"])
            if not state_only:
                bo = nb()
                for kc in range(KC):
                    self.mm(ps[bo][:], lhs(kc), win[:, kc, 1536:2048], kc == 0, kc == KC - 1, R=["win", "hn"], W=[("ps", bo)])
                self.tt("dve", tmpo, ps[bo][:], b_tok[:, 512:1024], ALU.add, R=[("ps", bo), "crow"], W=["hmo"])
                self.act(osig, tmpo, AF.Sigmoid, R=["hmo"], W=["osig"])

        e1, nlf, gm, gw, wk, dec = (smx[:, 0:4], smx[:, 4:8], smx[:, 8:12], smx[:, 12:16], smx[:, 16:20], smx[:, 20:24])
        den, rden, ss, rstd = smx[:, 24:28], smx[:, 28:32], smx[:, 32:36], smx[:, 36:40]
        tri4 = self.tri_f

        def mlstm_chunk(i, state_only, vi):
            va = vaug[vi]
            vres = ("vaug", vi)
            tc_ = slice(i * 128, (i + 1) * 128)
            b7 = nb()
            self.act(e1, ifg[:, 4:8], AF.Exp, R=["ifg"], W=["e1"], scale=-1.0)
            self.act(nlf, e1, AF.Ln, R=["e1", "cvec"], W=["nlf"], bias=self.cv("one"))
            self.mm(ps[b7][:, 0:4], self.tri_f, nlf, True, True, R=["cmat", "nlf"], W=[("ps", b7)])
            self.mm(ps[b7][:, 4:8], self.ones_f, nlf, True, True, R=["cmat", "nlf"], W=[("ps", b7)])
            self.tt("dve", gm, ifg[:, 0:4], ps[b7][:, 0:4], ALU.add, R=["ifg", ("ps", b7)], W=["gm"])
            self.tt("dve", gw, gm, ps[b7][:, 4:8], ALU.subtract, R=["gm", ("ps", b7)], W=["gw"])
            self.act(wk, gw, AF.Exp, R=["gw"], W=["wk"])
            self.act(dec, ps[b7][:, 4:8], AF.Exp, R=[("ps", b7)], W=["dec"], scale=-1.0)
            if not state_only:
                for h in range(4):
                    self.ts("dve", trilf[:, h * 128:(h + 1) * 128], self.tri_f, nlf[:, h:h + 1], None, ALU.mult,
                            R=["cmat", "nlf"], W=["hmo"])
                b6, b5 = nb(), nb()
                self.mm(ps[b6][:], self.ones_f, trilf, True, True, R=["cmat", "hmo"], W=[("ps", b6)])
                for h in range(4):
                    self.act(P_t[:, h * 128:(h + 1) * 128], ps[b6][:, h * 128:(h + 1) * 128], AF.Exp,
                             R=[("ps", b6), "gm"], W=["P_t"], scale=-1.0, bias=gm[:, h:h + 1])
                self.act(erow, ps[b6][:], AF.Exp, R=[("ps", b6)], W=["erow"], scale=-1.0)
                for h in range(4):
                    self.mm(ps[b5][:, h * 128:(h + 1) * 128], kT[:, h, tc_], qT[:, h, tc_], True, True,
                            R=["kT", "qT"], W=[("ps", b5)])
                for h in range(4):
                    self.tt("dve", P_t[:, h * 128:(h + 1) * 128], P_t[:, h * 128:(h + 1) * 128], self.tri_f, ALU.mult,
                            R=["P_t", "cmat"], W=["P_t"])
                self.tt("dve", Sw, ps[b5][:], P_t, ALU.mult, R=[("ps", b5), "P_t"], W=["Sw"])
                self.tt("dve", qs.rearrange("p (h n) -> p h n", h=4), qT[:, :, tc_],
                        erow.rearrange("p (h n) -> p h n", h=4), ALU.mult, R=["qT", "erow"], W=["qs"])
                bo2 = [nb(), nb()]
                for h in range(4):
                    b = bo2[h // 2]
                    o_ = (h % 2) * 129
                    self.mm(ps[b][:, o_:o_ + 129], Sw[:, h * 128:(h + 1) * 128], va[:, h, :], True, False,
                            R=["Sw", vres], W=[("ps", b)])
                    self.mm(ps[b][:, o_:o_ + 129], qs[:, h * 128:(h + 1) * 128], Cbf[:, h, :], False, True,
                            R=["qs", "Cbf"], W=[("ps", b)])
                for b in range(2):
                    v3 = ps[bo2[b]][:, 0:258].rearrange("p (h n) -> p h n", h=2)
                    self.act(den[:, 2 * b:2 * b + 2], v3[:, :, 128], AF.Abs, R=[("ps", bo2[b])], W=["den"])
                self.ts("dve", den, den, SQRT_DH, None, ALU.max, R=["den"], W=["den"])
                self.recip(rden, den, R=["den"], W=["rden"])
                for h in range(4):
                    b = bo2[h // 2]
                    o_ = (h % 2) * 129
                    self.stt(hmo[:, h * 128:(h + 1) * 128], ps[b][:, o_:o_ + 128], rden[:, h:h + 1],
                             osig[:, h * 128:(h + 1) * 128], ALU.mult, ALU.mult,
                             R=[("ps", b), "rden", "osig"], W=["hmo"])
                self.act(P_t, hmo, AF.Square, R=["hmo"], W=["P_t"])
                self.S.op("dve", lambda e: e.tensor_reduce(ss, P_t.rearrange("p (h n) -> p h n", h=4), AX.X, ALU.add),
                          R=["P_t"], W=["ss"])
                self.act(rstd, ss, AF.Sqrt, R=["ss", "cvec"], W=["rstd"], scale=1.0 / 128, bias=self.cv("eps"))
                self.recip(rstd, rstd, R=["rstd"], W=["rstd"])
                for h in range(4):
                    self.stt(hmn[:, h * 128:(h + 1) * 128], hmo[:, h * 128:(h + 1) * 128], rstd[:, h:h + 1],
                             g_mh[:, h * 128:(h + 1) * 128], ALU.mult, ALU.mult, R=["hmo", "rstd", "crow"], W=["hmn"])
                bT = nb()
                pTb = ps[bT][:].bitcast(BF16)
                for h in range(4):
                    self.tr(pTb[:, h * 128:(h + 1) * 128], hmn[:, h * 128:(h + 1) * 128], self.ident_b[:],
                            R=["hmn", "ident_b"], W=[("ps", bT)])
                self.copy("act", hmT[:, :, tc_], pTb[:, 0:512].rearrange("p (h n) -> p h n", h=4),
                          R=[("ps", bT)], W=["hmT"])
            bK = nb()
            pKb = ps[bK][:].bitcast(BF16)
            for h in range(4):
                self.tr(pKb[:, h * 128:(h + 1) * 128], kT[:, h, tc_], self.ident_b[:], R=["kT", "ident_b"], W=[("ps", bK)])
            for h in range(4):
                self.ts("dve", kw[:, h * 128:(h + 1) * 128], pKb[:, h * 128:(h + 1) * 128], wk[:, h:h + 1], None, ALU.mult,
                        R=[("ps", bK), "wk"], W=["kw"])
            bu2 = [nb(), nb()]
            for h in range(4):
                b = bu2[h // 2]
                o_ = (h % 2) * 129
                self.mm(ps[b][:, o_:o_ + 129], kw[:, h * 128:(h + 1) * 128], va[:, h, :], True, True,
                        R=["kw", vres], W=[("ps", b)])
            for h in range(4):
                b = bu2[h // 2]
                o_ = (h % 2) * 129
                self.stt(Caug[:, h, :], Caug[:, h, :], dec[:, h:h + 1], ps[b][:, o_:o_ + 129], ALU.mult, ALU.add,
                         R=["state", "dec", ("ps", b)], W=["state"])
            self.copy("act", Cbf, Caug, R=["state"], W=["Cbf"])

        def run_pass(state_only):
            self.copy("pool", halo.rearrange("p j n -> p (j n)"), halo0.rearrange("p j n -> p (j n)"),
                      R=["xdst"], W=[("halo", j) for j in range(12)])
            vcnt = 0
            for tb in range(T // TM):
                t0 = tb * TM
                hr = self.hres(t0, t0 + TM)
                self.norm_stats(t0, TM, bank=nb())
                self.norm_apply(t0, TM, "g_mix" + L, hn3, "hn")
                for c in range(4):
                    acc, ares = conv_chunk("k", c, t0)
                    self.act(kT[:, c, :], acc, AF.Silu, R=[ares], W=["kT"])
                if not state_only:
                    for c in range(4):
                        acc, ares = conv_chunk("q", c, t0)
                        self.act(qT[:, c, :], acc, AF.Silu, R=[ares], W=["qT"])
                for i in range(TM // 128):
                    vi = vcnt % 2
                    vcnt += 1
                    tok_major(i, state_only, vi)
                    mlstm_chunk(i, state_only, vi)
                for c in range(4):
                    acc, ares = conv_chunk("x", c, t0)
                    rglru_chunk(c, acc, ares, state_only, t0)
                if not state_only:
                    for fo in range(KC):
                        bank = nb()
                        for kc in range(KC):
                            rhs = hmT[:, kc, :] if kc < 4 else hrn[:, kc - 4, :]
                            self.mm(ps[bank][:, :TM], wout[:, kc, fo * 128:(fo + 1) * 128], rhs, kc == 0, kc == KC - 1,
                                    R=["wout", "hmT", "hrn"], W=[("ps", bank)])
                        self.tt("dve", self.hT3[:, fo, t0:t0 + TM], self.hT3[:, fo, t0:t0 + TM], ps[bank][:, :TM], ALU.add,
                                R=hr + [("ps", bank)], W=hr)

        self.op("pool", lambda e: e.memset(state, 0.0), W=["state", "rstate"])
        self.op("pool", lambda e: e.memset(Cbf, 0.0), W=["Cbf"])
        if mode == "A":
            run_pass(True)
            o_state = self.dout("o_state", [128, 520])
            self.dma("sp", o_state[:, :], state, R=["state", "rstate"], W=["o_state"])
            return
        if mode == "B":
            i_state = self.din("i_state", [128, 520])
            self.dma("sp", state, i_state[:, :], W=["state", "rstate", "xdst"])
            self.copy("pool", Cbf, Caug, R=["xdst", "state"], W=["Cbf"])
        elif self.cfg.get("prepass", True):
            run_pass(True)
            self.S.barrier()
            self.copy("pool", xpack[:, 0:520], state, R=["state", "rstate"], W=["xsrc"])
            self.copy("pool", xpack[:, 520:556], tailb.rearrange("p j n -> p (j n)"), R=["xsrc"], W=["xsrc"])
            self.exchange(l, "s", xpack, 556, xrecv)
            self.copy("pool", state, xrecv[:, 0:520], R=["xdst"], W=["state", "rstate"])
            self.copy("pool", halo0.rearrange("p j n -> p (j n)"), xrecv[:, 520:556], R=["xdst"], W=["xdst"])
            self.copy("pool", Cbf, Caug, R=["state"], W=["Cbf"])
            self.S.barrier()
        run_pass(False)

    def program(self):
        cfg = self.cfg
        self.setup()
        if cfg["phases"] and cfg["phases"][0] == ("mixer", 0):
            self.mixer_prefetch(0)
        self.load_x()
        for pi, ph in enumerate(cfg["phases"]):
            if not (pi == 0 and ph[0] == "mixer"):
                self.S.barrier()
            if ph[0] == "ple":
                self.ple(ph[1])
            elif ph[0] == "ffn":
                self.ffn_dense()
            elif ph[0] == "moe":
                self.moe()
            elif ph[0] == "mixer":
                self.mixer(ph[1])
            elif ph[0] == "xchg":
                src = self.big[:, 20000:20036]
                dst = self.big[:, 20100:20136]
                self.copy("pool", src, self.cvec[:, 0:36], R=["cvec"], W=["xsrc"])
                self.exchange(ph[1], "x", src, 36, dst)
        if not (cfg["phases"] and cfg["phases"][-1][0] == "ple"):
            self.S.barrier()
        if cfg.get("mode") != "A":
            if cfg.get("final_norm", True):
                self.final_norm()
            self.store_out()
        if cfg.get("reorder", True):
            self.S.reorder(cfg.get("window", 48))
        self.S.finalize()
        with self.nc.Block() as block:
            self.S.emit(self.nc, block, self.stack)
        self.stack.close()
        return self.nc


FULL_CFG = {"phases": [("mixer", 0), ("ffn",), ("ple", 0), ("mixer", 1), ("moe",), ("ple", 1)]}


def make_in_maps(inputs, x_override=None, extra=None):
    f = lambda a: np.ascontiguousarray(np.asarray(a, np.float32))
    x = f(inputs["x"] if x_override is None else x_override).reshape(4 * 4096, D)
    p = f(inputs["p"]).reshape(2, 4 * 4096, DPLE)
    shared = {
        "w_in": f(inputs["w_in"]), "w_out": f(inputs["w_out"]),
        "w_ff_gate": f(inputs["w_ff_gate"][0]), "w_ff_up": f(inputs["w_ff_up"][0]),
        "w_ff_down": f(inputs["w_ff_down"][0]), "w_router": f(inputs["w_router"][0]),
        "w_e_gate": f(inputs["w_e_gate"][0]), "w_e_up": f(inputs["w_e_up"][0]),
        "w_e_down": f(inputs["w_e_down"][0]), "w_ple_gate": f(inputs["w_ple_gate"]),
        "w_ple_proj": f(inputs["w_ple_proj"]), "w_ra": f(inputs["w_ra"]), "w_ri": f(inputs["w_ri"]),
        "cmat": const_mats(),
    }
    maps = []
    for c in range(NCORES):
        cv, cr = pack_consts(inputs, c)
        m = dict(shared)
        m["x"] = np.ascontiguousarray(x[c * T:(c + 1) * T])
        m["p"] = np.ascontiguousarray(p[:, c * T:(c + 1) * T])
        m["cvec"] = cv
        m["crow"] = cr
        if extra is not None:
            m.update(extra[c])
        maps.append(m)
    return maps


_NC_CACHE = {}


def run(inputs, cfg, trace=False, x_override=None, extra=None):
    key = repr(sorted((k, repr(v)) for k, v in cfg.items()))
    if key not in _NC_CACHE:
        b = Builder(dict(cfg))
        _NC_CACHE[key] = b.program()
        _NC_CACHE[key + "#names"] = set(b.dram.keys())
    nc = _NC_CACHE[key]
    maps = make_in_maps(inputs, x_override, extra)
    names = _NC_CACHE[key + "#names"]
    maps = [{k: v for k, v in m.items() if k in names} for m in maps]
    res = run_bass_kernel_spmd(nc, maps, core_ids=list(range(NCORES)), **({"trace": True} if trace else {}))
    if cfg.get("mode") == "A":
        return None, res
    out = np.concatenate([np.asarray(r["out"], np.float32) for r in res.results], axis=0)
    return out.reshape(4, 4096, D), res


def _carry(resA):
    extra = []
    for c in range(NCORES):
        if c % 2 == 1:
            extra.append({"i_tail": np.asarray(resA.results[c - 1]["o_tail"], np.float32),
                          "i_state": np.asarray(resA.results[c - 1]["o_state"], np.float32)})
        else:
            extra.append({"i_tail": np.zeros((128, 36), np.float32), "i_state": np.zeros((128, 520), np.float32)})
    return extra


def kernel_unfused(inputs):
    _, rA0 = run(inputs, {"phases": [("mixer", 0)], "mode": "A"})
    h1, _ = run(inputs, {"phases": [("mixer", 0), ("ffn",), ("ple", 0)], "mode": "B", "final_norm": False},
                extra=_carry(rA0))
    _, rA1 = run(inputs, {"phases": [("mixer", 1)], "mode": "A"}, x_override=h1)
    out, _ = run(inputs, {"phases": [("mixer", 1), ("moe",), ("ple", 1)], "mode": "B", "final_norm": True},
                 x_override=h1, extra=_carry(rA1))
    return out


def kernel(**inputs):
    out, _ = run(inputs, FULL_CFG)
    return out
```
